# Optimizing a Trainium2 kernel written in Bass

```python
import math
import jax
import jax.numpy as jnp
from jax import lax
import numpy as np

D_MODEL = 1024
BATCH = 8
SEQ = 2048
DEPTH = 2

CHUNK = 64

D_A = D_MODEL // 2
S5_H = 16
S5_G = D_A // S5_H
S5_P = 64
DT_MIN = 1e-3
DT_MAX = 1e-1
D_B = D_MODEL - D_A
POOL_WINDOWS = (2, 4, 8, 16)
POOL_GROUPS = len(POOL_WINDOWS)
POOL_C = D_B // POOL_GROUPS
SB_HEADS = 16
SB_HEAD_DIM = D_MODEL // SB_HEADS
SB_BLOCK = 128
N_EXPERTS = 16
N_EXPERT_GROUPS = 4
EXPERTS_PER_GROUP = N_EXPERTS // N_EXPERT_GROUPS
TOP_K = 2
D_EXPERT = 512
PLE_DIM = 256
DEEPNORM_ALPHA = (2 * DEPTH) ** 0.25
DEEPNORM_BETA = (8 * DEPTH) ** -0.25
LN_EPS = 1e-5
N_EVEN = (DEPTH + 1) // 2
N_ODD = DEPTH // 2

kernel_name = 'hybrid_s5_pool_stickbreak_moe_trunk'

F32 = jnp.float32


def layer_norm(x, g, b):
    x32 = x.astype(F32)
    mu = jnp.mean(x32, axis=-1, keepdims=True)
    var = jnp.mean(jnp.square(x32 - mu), axis=-1, keepdims=True)
    return ((x32 - mu) * lax.rsqrt(var + LN_EPS) * g.astype(F32) + b.astype(F32)).astype(x.dtype)


def _complex_linear_combine(e1, e2):
    a1r, a1i, b1r, b1i = e1
    a2r, a2i, b2r, b2i = e2
    ar = a2r * a1r - a2i * a1i
    ai = a2r * a1i + a2i * a1r
    br = a2r * b1r - a2i * b1i + b2r
    bi = a2r * b1i + a2i * b1r + b2i
    return (ar, ai, br, bi)


def s5_mixer(u, lam_re, lam_im, log_dt, b_re, b_im, c_re, c_im, d_skip, w_glu):
    bsz, s, _ = u.shape
    u32 = u.astype(F32).reshape(bsz, s, S5_G, S5_H)
    lam_re = lam_re.astype(F32)
    lam_im = lam_im.astype(F32)
    dt = jnp.exp(log_dt.astype(F32))[:, None]
    mag = jnp.exp(lam_re * dt)
    ang = lam_im * dt
    lb_re = mag * jnp.cos(ang)
    lb_im = mag * jnp.sin(ang)
    den = lam_re * lam_re + lam_im * lam_im
    num_re = lb_re - 1.0
    f_re = (num_re * lam_re + lb_im * lam_im) / den
    f_im = (lb_im * lam_re - num_re * lam_im) / den
    b_re = b_re.astype(F32)
    b_im = b_im.astype(F32)
    bb_re = f_re[..., None] * b_re - f_im[..., None] * b_im
    bb_im = f_re[..., None] * b_im + f_im[..., None] * b_re
    bu_re = jnp.einsum('bsgh,gph->sbgp', u32, bb_re)
    bu_im = jnp.einsum('bsgh,gph->sbgp', u32, bb_im)
    a_re = jnp.broadcast_to(lb_re[None, None], (s, 1, S5_G, S5_P))
    a_im = jnp.broadcast_to(lb_im[None, None], (s, 1, S5_G, S5_P))
    _, _, st_re, st_im = lax.associative_scan(
        _complex_linear_combine, (a_re, a_im, bu_re, bu_im), axis=0)
    y = (jnp.einsum('sbgp,ghp->bsgh', st_re, c_re.astype(F32))
         - jnp.einsum('sbgp,ghp->bsgh', st_im, c_im.astype(F32))
         + d_skip.astype(F32) * u32)
    y = jax.nn.gelu(y.reshape(bsz, s, D_A))
    out = y * jax.nn.sigmoid(y @ w_glu.astype(F32))
    return out.astype(u.dtype)


def pool_mixer(v, pool_w, pool_scale):
    bsz, s, _ = v.shape
    v32 = v.astype(F32).reshape(bsz, s, POOL_GROUPS, POOL_C)
    csum = jnp.cumsum(v32, axis=1)
    t = jnp.arange(s)
    means = []
    for gi, w in enumerate(POOL_WINDOWS):
        c = csum[:, :, gi]
        lagged = jnp.pad(c, ((0, 0), (w, 0), (0, 0)))[:, :s]
        count = jnp.minimum(t + 1, w).astype(F32)[None, :, None]
        means.append((c - lagged) / count)
    pooled = jnp.stack(means, axis=2) - v32
    mixed = jnp.einsum('bsgc,gcd->bsgd', pooled, pool_w.astype(F32))
    return (mixed.reshape(bsz, s, D_B) * pool_scale.astype(F32)).astype(v.dtype)


def stick_breaking_attention(qkv):
    bsz, s, _ = qkv.shape
    q, k, v = jnp.split(qkv.astype(F32), 3, axis=-1)
    to_heads = lambda z: z.reshape(bsz, s, SB_HEADS, SB_HEAD_DIM).transpose(0, 2, 1, 3)
    q, k, v = to_heads(q), to_heads(k), to_heads(v)
    scale = SB_HEAD_DIM ** -0.5
    outs = []
    for blk in range(s // SB_BLOCK):
        q0 = blk * SB_BLOCK
        end = q0 + SB_BLOCK
        z = jnp.einsum('bhqd,bhkd->bhqk', q[:, :, q0:end], k[:, :, :end]) * scale
        t_pos = q0 + jnp.arange(SB_BLOCK)
        s_pos = jnp.arange(end)
        causal = s_pos[None, :] < t_pos[:, None]
        log_keep = jnp.where(causal, jax.nn.log_sigmoid(-z), 0.0)
        log_after = lax.cumsum(log_keep, axis=3, reverse=True) - log_keep
        wts = jnp.where(causal, jnp.exp(jax.nn.log_sigmoid(z) + log_after), 0.0)
        outs.append(jnp.einsum('bhqk,bhkd->bhqd', wts, v[:, :, :end]))
    o = jnp.concatenate(outs, axis=2)
    return o.transpose(0, 2, 1, 3).reshape(bsz, s, D_MODEL).astype(qkv.dtype)


def grouped_moe(x, router_w, router_bias, w1, w3, w2):
    bsz, s, d = x.shape
    xt = x.reshape(-1, d)
    scores = jax.nn.sigmoid((xt @ router_w).astype(F32))
    sel = scores + router_bias.astype(F32)
    grp = sel.reshape(-1, N_EXPERT_GROUPS, EXPERTS_PER_GROUP)
    grp_score = jnp.sum(lax.top_k(grp, TOP_K)[0], axis=-1)
    top_group = jnp.argmax(grp_score, axis=-1)
    in_group = (jnp.arange(N_EXPERTS) // EXPERTS_PER_GROUP)[None, :] == top_group[:, None]
    _, idx = lax.top_k(jnp.where(in_group, sel, -jnp.inf), TOP_K)
    w = jnp.take_along_axis(scores, idx, axis=-1)
    w = w / jnp.sum(w, axis=-1, keepdims=True)
    gates = jnp.sum(jax.nn.one_hot(idx, N_EXPERTS, dtype=F32) * w[..., None], axis=1)
    h = jax.nn.silu(jnp.einsum('td,edf->etf', xt, w1)) * jnp.einsum('td,edf->etf', xt, w3)
    h = h * gates.T[:, :, None].astype(h.dtype)
    y = jnp.einsum('etf,efd->td', h, w2)
    return y.reshape(bsz, s, d).astype(x.dtype)


def setup_inputs(seed: int = 0) -> dict:
    key = jax.random.key(seed)
    ks = jax.random.split(key, 32)
    nrm = lambda k, shape, sc: jax.random.normal(k, shape, F32) * sc
    n_idx = jnp.arange(S5_P, dtype=F32)
    return {
        'x': nrm(ks[0], (BATCH, SEQ, D_MODEL), 1.0),
        'p': nrm(ks[1], (DEPTH, BATCH, SEQ, PLE_DIM), 1.0),
        'ab_w_in': nrm(ks[2], (N_EVEN, D_MODEL, D_A + D_B), D_MODEL ** -0.5),
        's5_lambda_re': -0.5 + nrm(ks[3], (N_EVEN, S5_G, S5_P), 0.01),
        's5_lambda_im': math.pi * n_idx + nrm(ks[4], (N_EVEN, S5_G, S5_P), 0.01),
        's5_log_dt': jax.random.uniform(ks[5], (N_EVEN, S5_G), F32, math.log(DT_MIN), math.log(DT_MAX)),
        's5_b_re': nrm(ks[6], (N_EVEN, S5_G, S5_P, S5_H), (2 * S5_H) ** -0.5),
        's5_b_im': nrm(ks[7], (N_EVEN, S5_G, S5_P, S5_H), (2 * S5_H) ** -0.5),
        's5_c_re': nrm(ks[8], (N_EVEN, S5_G, S5_H, S5_P), S5_P ** -0.5),
        's5_c_im': nrm(ks[9], (N_EVEN, S5_G, S5_H, S5_P), S5_P ** -0.5),
        's5_d': nrm(ks[10], (N_EVEN, S5_G, S5_H), 1.0),
        's5_w_glu': nrm(ks[11], (N_EVEN, D_A, D_A), D_A ** -0.5),
        'pool_w': nrm(ks[12], (N_EVEN, POOL_GROUPS, POOL_C, POOL_C), POOL_C ** -0.5),
        'pool_scale': 1.0 + nrm(ks[13], (N_EVEN, D_B), 0.1),
        'ab_w_out': nrm(ks[14], (N_EVEN, D_A + D_B, D_MODEL), (D_A + D_B) ** -0.5 * DEEPNORM_BETA),
        'sb_w_qkv': nrm(ks[15], (N_ODD, D_MODEL, 3 * D_MODEL), D_MODEL ** -0.5),
        'sb_w_out': nrm(ks[16], (N_ODD, D_MODEL, D_MODEL), D_MODEL ** -0.5 * DEEPNORM_BETA),
        'ln_mix_g': 1.0 + nrm(ks[17], (DEPTH, D_MODEL), 0.01),
        'ln_mix_b': nrm(ks[18], (DEPTH, D_MODEL), 0.01),
        'ln_ffn_g': 1.0 + nrm(ks[19], (DEPTH, D_MODEL), 0.01),
        'ln_ffn_b': nrm(ks[20], (DEPTH, D_MODEL), 0.01),
        'router_w': nrm(ks[21], (D_MODEL, N_EXPERTS), D_MODEL ** -0.5),
        'router_bias': nrm(ks[22], (N_EXPERTS,), 0.01),
        'moe_w1': nrm(ks[23], (DEPTH, N_EXPERTS, D_MODEL, D_EXPERT), D_MODEL ** -0.5),
        'moe_w3': nrm(ks[24], (DEPTH, N_EXPERTS, D_MODEL, D_EXPERT), D_MODEL ** -0.5),
        'moe_w2': nrm(ks[25], (DEPTH, N_EXPERTS, D_EXPERT, D_MODEL), D_EXPERT ** -0.5 * DEEPNORM_BETA),
        'ple_w_proj': nrm(ks[26], (DEPTH, PLE_DIM, D_MODEL), PLE_DIM ** -0.5 * DEEPNORM_BETA),
        'ple_w_gate': nrm(ks[27], (DEPTH, D_MODEL, D_MODEL), D_MODEL ** -0.5),
    }


def reference(x, p, ab_w_in, s5_lambda_re, s5_lambda_im, s5_log_dt, s5_b_re, s5_b_im,
              s5_c_re, s5_c_im, s5_d, s5_w_glu, pool_w, pool_scale, ab_w_out,
              sb_w_qkv, sb_w_out, ln_mix_g, ln_mix_b, ln_ffn_g, ln_ffn_b,
              router_w, router_bias, moe_w1, moe_w3, moe_w2, ple_w_proj, ple_w_gate):
    for i in range(DEPTH):
        j = i // 2
        if i % 2 == 0:
            h = x @ ab_w_in[j]
            out_a = s5_mixer(h[..., :D_A], s5_lambda_re[j], s5_lambda_im[j], s5_log_dt[j],
                             s5_b_re[j], s5_b_im[j], s5_c_re[j], s5_c_im[j], s5_d[j], s5_w_glu[j])
            out_b = pool_mixer(h[..., D_A:], pool_w[j], pool_scale[j])
            mix = jnp.concatenate([out_a, out_b], axis=-1) @ ab_w_out[j]
        else:
            mix = stick_breaking_attention(x @ sb_w_qkv[j]) @ sb_w_out[j]
        x = layer_norm(DEEPNORM_ALPHA * x + mix, ln_mix_g[i], ln_mix_b[i])
        ple = (p[i] @ ple_w_proj[i]) * jax.nn.sigmoid(x @ ple_w_gate[i])
        ffn = grouped_moe(x, router_w, router_bias, moe_w1[i], moe_w3[i], moe_w2[i])
        x = layer_norm(DEEPNORM_ALPHA * x + ffn + ple, ln_ffn_g[i], ln_ffn_b[i])
    return x
```

```python
import math
import os
DBG = os.environ.get('KDBG', '')
from contextlib import ExitStack

import numpy as np
import concourse.bass as bass
import concourse.mybir as mybir
from concourse.bass_utils import run_bass_kernel_spmd

F32 = mybir.dt.float32
BF16 = mybir.dt.bfloat16
I32 = mybir.dt.int32
AF = mybir.ActivationFunctionType
ALU = mybir.AluOpType
AX = mybir.AxisListType

S = 2048
D = 1024
NT = 16
KC = 8
NE = 16
DE = 512
PLE = 256
ALPHA = 4.0 ** 0.25
LN_EPS = 1e-5
ENGS = ("tensor", "vector", "scalar", "gpsimd", "sync")
NSEM = 48


class Sig:
    def __init__(self, k, idx):
        self.k = k
        self.idx = idx

    @property
    def sem(self):
        return self.k.sems[self.idx]

    @property
    def val(self):
        return self.k.counts[self.idx]

    def post(self, n=1):
        self.k.counts[self.idx] += n
        return self.k.counts[self.idx]


class Phase:
    def __init__(self, k, name):
        self.k = k
        self.name = name
        self.ops = {e: [] for e in ENGS}
        self.selfsig = {}
        self.waited = {}
        k.next_sem = 0

    def sig(self):
        i = self.k.next_sem
        self.k.next_sem += 1
        assert i < NSEM, "out of semaphores"
        return Sig(self.k, i)

    def do(self, eng, fn, post=None, n=None):
        if post is not None:
            if n is None:
                n = 1
            v = post.post(n)
            sem = post.sem
            self.ops[eng].append(lambda e, fn=fn, sem=sem, n=n: fn(e).then_inc(sem, n))
            return v
        self.ops[eng].append(fn)
        return None

    def dos(self, eng, fn):
        if eng not in self.selfsig:
            self.selfsig[eng] = self.sig()
        sg = self.selfsig[eng]
        v = self.do(eng, fn, post=sg)
        self.wait(eng, sg, v)
        return v

    def dma(self, eng, out, in_, post):
        return self.do(eng, lambda e, out=out, in_=in_: e.dma_start(out=out, in_=in_), post=post, n=16)

    def wait(self, eng, sig, v=None):
        if v is None:
            v = sig.val
        if v <= 0:
            return
        key = (eng, sig.idx)
        if self.waited.get(key, 0) >= v:
            return
        self.waited[key] = v
        sem = sig.sem
        self.ops[eng].append(lambda e, sem=sem, v=v: e.wait_ge(sem, v))

    def emit(self):
        nc = self.k.nc
        with nc.Block() as b:
            for eng in ENGS:
                lst = self.ops[eng]
                if not lst:
                    continue

                def body(e, lst=lst):
                    for f in lst:
                        f(e)

                getattr(b, eng)(body)


class K:
    def __init__(self, nc, es):
        self.nc = nc
        self.es = es
        self.sems = [es.enter_context(nc.semaphore(f"sm{i}")) for i in range(NSEM)]
        self.counts = [0] * NSEM
        self.next_sem = 0

    def sb(self, es, name, shape, dt):
        self.uid = getattr(self, "uid", 0) + 1
        return es.enter_context(self.nc.sbuf_tensor(f"{name}_u{self.uid}", list(shape), dt))


def transpose_tiles(k, ph, X, XT, tiles, ready, banks, ident, done_sig=None):
    psf_e = {"scalar": ph.sig(), "vector": ph.sig()}
    pst = ph.sig()
    evs = []
    for n, t in enumerate(tiles):
        j = n % 2
        if t in ready:
            ph.wait("tensor", ready[t][0], ready[t][1])
        if n >= 2:
            ph.wait("tensor", evs[n - 2][0], evs[n - 2][1])
        for c in range(KC):
            bank = banks[2 * j + c // 4]
            out = bank[:, (c % 4) * 128:(c % 4 + 1) * 128]
            fn = lambda e, out=out, in_=X[:, t, c * 128:(c + 1) * 128]: e.transpose(out, in_, ident[:])
            if c == KC - 1:
                v = ph.do("tensor", fn, post=pst)
            else:
                ph.do("tensor", fn)
        eng = "scalar" if n % 2 == 0 else "vector"
        ph.wait(eng, pst, v)
        for hb in range(2):
            bank = banks[2 * j + hb]
            out = XT[:, hb * 4:(hb + 1) * 4, t * 128:(t + 1) * 128]
            in_ = bank[:, :].rearrange("p (c n) -> p c n", c=4)
            if eng == "scalar":
                fn = lambda e, out=out, in_=in_: e.activation(out=out, in_=in_, func=AF.Copy)
            else:
                fn = lambda e, out=out, in_=in_: e.tensor_copy(out=out, in_=in_)
            if hb == 1:
                evs.append((psf_e[eng], ph.do(eng, fn, post=psf_e[eng])))
            else:
                ph.do(eng, fn)
    return psf_e, evs


def ln_tiles(k, ph, X, tiles, ready, gb, stats, eps_t):
    s_st = ph.sig()
    s_sq = ph.sig()
    s_act = ph.sig()
    s_pool = ph.sig()
    s_dvgb = ph.sig()
    out_ready = {}
    act_vals = {}
    sq_vals = {}
    deferred = []
    nt = len(tiles)

    def stage_a(n):
        t = tiles[n]
        if t in ready:
            ph.wait("vector", ready[t][0], ready[t][1])
        if n >= 4:
            ph.wait("vector", s_act, act_vals[n - 4])
        st = stats[:, n % 4, :]
        for hb in range(2):
            ph.dos("vector", lambda e, o=st[:, hb * 6:(hb + 1) * 6], i=X[:, t, hb * 512:(hb + 1) * 512]: e.bn_stats(out=o, in_=i))
        va = ph.do("vector", lambda e, o=st[:, 12:14], i=st[:, 0:12]: e.bn_aggr(out=o, in_=i), post=s_st)
        ph.wait("scalar", s_st, va)
        sq_vals[n] = ph.do("scalar", lambda e, o=st[:, 14:15], i=st[:, 13:14]: e.activation(out=o, in_=i, func=AF.Sqrt, bias=eps_t[:, 0:1], scale=1.0), post=s_sq)

    def stage_b(n):
        t = tiles[n]
        st = stats[:, n % 4, :]
        rstd = st[:, 14:15]
        nmr = st[:, 15:16]
        ph.wait("vector", s_sq, sq_vals[n])
        ph.dos("vector", lambda e, o=rstd: e.reciprocal(out=o, in_=o))
        v = ph.do("vector", lambda e, o=nmr, i=st[:, 12:13], r=rstd: e.scalar_tensor_tensor(out=o, in0=i, scalar=-1.0, in1=r, op0=ALU.mult, op1=ALU.mult), post=s_st)
        ph.wait("scalar", s_st, v)
        v = ph.do("scalar", lambda e, o=X[:, t, :], r=rstd, b=nmr: e.activation(out=o, in_=o, func=AF.Identity, bias=b, scale=r), post=s_act)
        act_vals[n] = v
        if n % 2 == 0:
            ph.wait("gpsimd", s_act, v)
            ph.dos("gpsimd", lambda e, o=X[:, t, :], g=gb[:, 0, :]: e.tensor_tensor(out=o, in0=o, in1=g, op=ALU.mult))
            v2 = ph.do("gpsimd", lambda e, o=X[:, t, :], g=gb[:, 1, :]: e.tensor_tensor(out=o, in0=o, in1=g, op=ALU.add), post=s_pool)
            out_ready[t] = (s_pool, v2)
        else:
            def gbops(t=t, v=v):
                ph.wait("vector", s_act, v)
                ph.dos("vector", lambda e, o=X[:, t, :], g=gb[:, 0, :]: e.tensor_tensor(out=o, in0=o, in1=g, op=ALU.mult))
                v2 = ph.do("vector", lambda e, o=X[:, t, :], g=gb[:, 1, :]: e.tensor_tensor(out=o, in0=o, in1=g, op=ALU.add), post=s_dvgb)
                out_ready[t] = (s_dvgb, v2)
            deferred.append(gbops)

    stage_a(0)
    for n in range(nt):
        if n + 1 < nt:
            stage_a(n + 1)
        stage_b(n)
        if n % 2 == 0 and deferred:
            deferred.pop(0)()
    while deferred:
        deferred.pop(0)()
    return out_ready, s_act


def bcast_row(dram_vec_ap, n):
    return dram_vec_ap.partition_broadcast(128)


def build_program(nc, plan=("mix0", "ffn0", "mix1", "ffn1")):
    dt_in = {}

    def din(name, shape):
        dt_in[name] = nc.dram_tensor(name, list(shape), F32, kind="ExternalInput").ap()
        return dt_in[name]

    x_d = din("x", [S, D])
    p_d = din("p", [2, S, PLE])
    ab_w_in = din("ab_w_in", [D, D])
    s5_lre = din("s5_lambda_re", [32, 64])
    s5_lim = din("s5_lambda_im", [32, 64])
    s5_ldt = din("s5_log_dt", [32])
    s5_bre = din("s5_b_re", [32, 64, 16])
    s5_bim = din("s5_b_im", [32, 64, 16])
    s5_cre = din("s5_c_re", [32, 16, 64])
    s5_cim = din("s5_c_im", [32, 16, 64])
    s5_d = din("s5_d", [512])
    s5_wglu = din("s5_w_glu", [512, 512])
    pool_w = din("pool_w", [4, 128, 128])
    pool_scale = din("pool_scale", [512])
    ab_w_out = din("ab_w_out", [D, D])
    sb_w_qkv = din("sb_w_qkv", [D, 3 * D])
    sb_w_out = din("sb_w_out", [D, D])
    ln_mix_g = din("ln_mix_g", [2, D])
    ln_mix_b = din("ln_mix_b", [2, D])
    ln_ffn_g = din("ln_ffn_g", [2, D])
    ln_ffn_b = din("ln_ffn_b", [2, D])
    router_w = din("router_w", [D, NE])
    router_bias = din("router_bias", [NE])
    moe_w1 = din("moe_w1", [2, NE, D, DE])
    moe_w3 = din("moe_w3", [2, NE, D, DE])
    moe_w2 = din("moe_w2", [2, NE, DE, D])
    ple_w_proj = din("ple_w_proj", [2, PLE, D])
    ple_w_gate = din("ple_w_gate", [2, D, D])
    y_d = nc.dram_tensor("y", [S, D], F32, kind="ExternalOutput").ap()

    with ExitStack() as es:
        k = K(nc, es)
        X = k.sb(es, "X", [128, NT, D], F32)
        XT = k.sb(es, "XT", [128, KC, S], BF16)
        ident = k.sb(es, "ident", [128, 128], F32)
        gb = k.sb(es, "gb", [128, 2, D], F32)
        stats = k.sb(es, "stats", [128, 4, 16], F32)
        k.eps_t = k.sb(es, "eps_t", [128, 1], F32)
        psall = es.enter_context(nc.psum_tensor("psall", [128, 8, 512], F32))
        k.psall = psall
        banks = [psall[:, i, :] for i in range(8)]

        ph = Phase(k, "load")
        s_ld = ph.sig()
        s_id = ph.sig()
        ph.dos("gpsimd", lambda e: e.memset(ident[:], 0.0))
        ph.dos("gpsimd", lambda e: e.memset(k.eps_t[:], LN_EPS))
        ph.do("gpsimd", lambda e: e.affine_select(out=ident[:], in_=ident[:], pattern=[[-1, 128]], compare_op=ALU.not_equal,
                                                   fill=1.0, base=0, channel_multiplier=1), post=s_id)
        ready = {}
        xv = x_d.rearrange("(t p) d -> p t d", p=128)
        for q in range(4):
            sq = ph.sig()
            for t in range(q * 4, q * 4 + 4):
                v = ph.dma("sync", X[:, t, :], xv[:, t, :], sq)
            for t in range(q * 4, q * 4 + 4):
                ready[t] = (sq, v)
        ph.wait("tensor", s_id)
        transpose_tiles(k, ph, X, XT, list(range(NT)), ready, banks[0:4], ident)
        ph.emit()
        if 'dumpxt' in DBG:
            dbg_d = nc.dram_tensor("dbg", [128, KC * S], F32, kind="ExternalOutput").ap()
            ph = Phase(k, "dump")
            sd = ph.sig()
            for c in range(KC):
                ph.dma("gpsimd", dbg_d[:, c * S:(c + 1) * S], XT[:, c, :], sd)
            ph.wait("gpsimd", sd)
            ph.emit()

        for step in plan:
            layer = int(step[-1])
            if step.startswith("mix"):
                if layer == 0:
                    mixer_s5_pool(k, X, XT, ident, gb, stats, banks, dt_in)
                else:
                    mixer_attention(k, X, XT, ident, gb, stats, banks, dt_in)
            else:
                ffn_layer(k, layer, X, XT, ident, gb, stats, banks, dt_in, last=(step == plan[-1]))

        ph = Phase(k, "store")
        s_st = ph.sig()
        yv = y_d.rearrange("(t p) d -> p t d", p=128)
        for t in range(NT):
            ph.dma("sync", yv[:, t, :], X[:, t, :], s_st)
        ph.wait("sync", s_st)
        ph.emit()
    return nc


def load_gb(ph, gb, g_d, b_d, sig):
    ph.dma("sync", gb[:, 0, :], g_d.partition_broadcast(128), sig)
    return ph.dma("sync", gb[:, 1, :], b_d.partition_broadcast(128), sig)


def ln_phase(k, X, XT, ident, gb, stats, banks, g_d, b_d, do_transpose):
    ph = Phase(k, "ln")
    s_gb = ph.sig()
    load_gb(ph, gb, g_d, b_d, s_gb)
    ph.wait("gpsimd", s_gb)
    ph.wait("vector", s_gb)
    ready, _ = ln_tiles(k, ph, X, list(range(NT)), {}, gb, stats, k.eps_t)
    ph.emit()
    if do_transpose:
        ph = Phase(k, "lnT")
        transpose_tiles(k, ph, X, XT, list(range(NT)), {}, banks[0:4], ident)
        ph.emit()


def ffn_layer(k, layer, X, XT, ident, gb, stats, banks, dt_in, last):
    nc = k.nc
    p_d = dt_in["p"][layer]
    wp_d = dt_in["ple_w_proj"][layer]
    wg_d = dt_in["ple_w_gate"][layer]
    wr_d = dt_in["router_w"]
    rb_d = dt_in["router_bias"]
    w1_d = dt_in["moe_w1"][layer]
    w3_d = dt_in["moe_w3"][layer]
    w2_d = dt_in["moe_w2"][layer]

    with ExitStack() as es:
        gates = k.sb(es, "gates", [128, NT, NE], F32)
        with ExitStack() as es1:
            wg = k.sb(es1, "wg", [128, KC, D], BF16)
            wp = k.sb(es1, "wp", [128, 2, D], BF16)
            wr = k.sb(es1, "wr", [128, KC, NE], BF16)
            rb = k.sb(es1, "rb", [128, NE], F32)
            pt = k.sb(es1, "pt", [128, NT, PLE], F32)
            pT = k.sb(es1, "pT", [128, 2, 2, 128], BF16)
            sg = k.sb(es1, "sg", [128, 2, 512], F32)
            tmp = k.sb(es1, "tmp", [128, 2, 512], F32)
            sc = k.sb(es1, "sc", [128, NT, NE], F32)
            rt = k.sb(es1, "rt", [128, 8, NT * NE], F32)

            ph = Phase(k, "ffn1")
            s_w = ph.sig()
            s_p = ph.sig()
            ph.dma("gpsimd", wg[:], wg_d.rearrange("(c p) d -> p c d", p=128), s_w)
            ph.dma("gpsimd", wp[:], wp_d.rearrange("(c p) d -> p c d", p=128), s_w)
            ph.dma("gpsimd", wr[:], wr_d.rearrange("(c p) d -> p c d", p=128), s_w)
            ph.dma("sync", rb[:], rb_d.partition_broadcast(128), s_w)
            pv = p_d.rearrange("(t p) d -> p t d", p=128)
            p_ready = []
            for q in range(4):
                sq = ph.sig()
                p_ready.append((sq, ph.dma("sync", pt[:, q * 4:(q + 1) * 4, :], pv[:, q * 4:(q + 1) * 4, :], sq)))
            ph.wait("tensor", s_w)

            s_tp = ph.sig()
            s_tc = ph.sig()
            s_mm = ph.sig()
            s_sg = ph.sig()
            s_dv = ph.sig()
            s_r = ph.sig()
            bT = [banks[0], banks[6]]
            bR = banks[1]
            bP = [banks[2], banks[3]]
            bG = [banks[4], banks[5]]
            tc_vals = []
            dv_vals = []
            n_it = 0
            for t in range(NT):
                j = t % 2
                ph.wait("tensor", p_ready[t // 4][0], p_ready[t // 4][1])
                if t >= 2:
                    ph.wait("tensor", s_tc, tc_vals[t - 2])
                for c in range(2):
                    fn = lambda e, o=bT[j][:, c * 128:(c + 1) * 128], i=pt[:, t, c * 128:(c + 1) * 128]: e.transpose(o, i, ident[:])
                    v = ph.do("tensor", fn, post=s_tp) if c == 1 else ph.do("tensor", fn)
                ph.wait("scalar", s_tp, v)
                vtc = ph.do("scalar", lambda e, o=pT[:, j, :, :], i=bT[j][:, 0:256].rearrange("p (c n) -> p c n", c=2): e.activation(out=o, in_=i, func=AF.Copy), post=s_tc)
                tc_vals.append(vtc)
                for c in range(KC):
                    fn = lambda e, o=bR[:, t * NE:(t + 1) * NE], l=XT[:, c, t * 128:(t + 1) * 128], r=wr[:, c, :], st=(c == 0), sp=(c == KC - 1): e.matmul(o, lhsT=l, rhs=r, start=st, stop=sp)
                    if c == KC - 1 and t == NT - 1:
                        ph.do("tensor", fn, post=s_r)
                    else:
                        ph.do("tensor", fn)
                ph.wait("tensor", s_tc, vtc)
                for hb in range(2):
                    jj = n_it % 2
                    if n_it >= 2:
                        ph.wait("tensor", s_dv, dv_vals[n_it - 2])
                    for c in range(2):
                        ph.do("tensor", lambda e, o=bP[jj][:, :], l=pT[:, j, c, :], r=wp[:, c, hb * 512:(hb + 1) * 512], st=(c == 0), sp=(c == 1): e.matmul(o, lhsT=l, rhs=r, start=st, stop=sp))
                    for c in range(KC):
                        fn = lambda e, o=bG[jj][:, :], l=XT[:, c, t * 128:(t + 1) * 128], r=wg[:, c, hb * 512:(hb + 1) * 512], st=(c == 0), sp=(c == KC - 1): e.matmul(o, lhsT=l, rhs=r, start=st, stop=sp)
                        v = ph.do("tensor", fn, post=s_mm) if c == KC - 1 else ph.do("tensor", fn)
                    ph.wait("scalar", s_mm, v)
                    if n_it >= 2:
                        ph.wait("scalar", s_dv, dv_vals[n_it - 2])
                    vs = ph.do("scalar", lambda e, o=sg[:, jj, :], i=bG[jj][:, :]: e.activation(out=o, in_=i, func=AF.Sigmoid), post=s_sg)
                    ph.wait("vector", s_sg, vs)
                    ph.dos("vector", lambda e, o=tmp[:, jj, :], a=bP[jj][:, :], b=sg[:, jj, :]: e.tensor_tensor(out=o, in0=a, in1=b, op=ALU.mult))
                    xs = X[:, t, hb * 512:(hb + 1) * 512]
                    vd = ph.do("vector", lambda e, o=xs, b=tmp[:, jj, :]: e.scalar_tensor_tensor(out=o, in0=o, scalar=ALPHA, in1=b, op0=ALU.mult, op1=ALU.add), post=s_dv)
                    dv_vals.append(vd)
                    n_it += 1
            ph.wait("scalar", s_r)
            s_sc = ph.sig()
            ph.do("scalar", lambda e: e.activation(out=sc[:].rearrange("p t e -> p (t e)"), in_=bR[:, 0:NT * NE], func=AF.Sigmoid), post=s_sc)
            ph.wait("vector", s_sc)
            ph.wait("vector", s_w)
            W = NT * NE
            G4 = NT * 4

            def v4(ap):
                return ap.rearrange("p (g e) -> p g e", e=4)

            sel = rt[:, 0, :]
            m1 = rt[:, 1, 0:G4]
            mk1 = rt[:, 2, :]
            sel2 = rt[:, 3, :]
            m2 = rt[:, 1, G4:2 * G4]
            mk2 = rt[:, 4, :]
            gs = rt[:, 1, 2 * G4:3 * G4]
            gm = rt[:, 1, 3 * G4:3 * G4 + NT]
            gmask = rt[:, 5, 0:G4]
            den = rt[:, 5, G4:G4 + NT]
            rden = rt[:, 5, G4 + NT:G4 + 2 * NT]
            gun = rt[:, 6, :]
            scf = sc[:].rearrange("p t e -> p (t e)")
            BIG = 1.0e4
            dv = lambda fn: ph.dos("vector", fn)
            dv(lambda e: e.tensor_tensor(out=sel.rearrange("p (t e) -> p t e", e=NE), in0=sc[:], in1=rb[:].unsqueeze(1).to_broadcast([128, NT, NE]), op=ALU.add))
            dv(lambda e: e.tensor_reduce(out=m1, in_=v4(sel), axis=AX.X, op=ALU.max))
            dv(lambda e: e.tensor_tensor(out=v4(mk1), in0=v4(sel), in1=m1.unsqueeze(2).to_broadcast([128, G4, 4]), op=ALU.is_ge))
            dv(lambda e: e.scalar_tensor_tensor(out=sel2, in0=mk1, scalar=-BIG, in1=sel, op0=ALU.mult, op1=ALU.add))
            dv(lambda e: e.tensor_reduce(out=m2, in_=v4(sel2), axis=AX.X, op=ALU.max))
            dv(lambda e: e.tensor_tensor(out=v4(mk2), in0=v4(sel2), in1=m2.unsqueeze(2).to_broadcast([128, G4, 4]), op=ALU.is_ge))
            dv(lambda e: e.tensor_tensor(out=gs, in0=m1, in1=m2, op=ALU.add))
            dv(lambda e: e.tensor_reduce(out=gm, in_=gs.rearrange("p (t g) -> p t g", g=4), axis=AX.X, op=ALU.max))
            dv(lambda e: e.tensor_tensor(out=gmask.rearrange("p (t g) -> p t g", g=4), in0=gs.rearrange("p (t g) -> p t g", g=4),
                                         in1=gm.unsqueeze(2).to_broadcast([128, NT, 4]), op=ALU.is_ge))
            dv(lambda e: e.tensor_tensor(out=mk1, in0=mk1, in1=mk2, op=ALU.add))
            dv(lambda e: e.tensor_tensor(out=v4(mk1), in0=v4(mk1), in1=gmask.unsqueeze(2).to_broadcast([128, G4, 4]), op=ALU.mult))
            dv(lambda e: e.tensor_tensor(out=gun, in0=mk1, in1=scf, op=ALU.mult))
            dv(lambda e: e.tensor_reduce(out=den, in_=gun.rearrange("p (t e) -> p t e", e=NE), axis=AX.X, op=ALU.add))
            dv(lambda e: e.reciprocal(out=rden, in_=den))
            dv(lambda e: e.tensor_tensor(out=gates[:], in0=gun.rearrange("p (t e) -> p t e", e=NE), in1=rden.unsqueeze(2).to_broadcast([128, NT, NE]), op=ALU.mult))
            ph.emit()

        with ExitStack() as es3:
          if 'noexp' not in DBG:
              w1 = [k.sb(es3, f"w1_{j}", [128, KC, DE], BF16) for j in range(2)]
              w3 = [k.sb(es3, f"w3_{j}", [128, KC, DE], BF16) for j in range(2)]
              w2 = [k.sb(es3, f"w2_{j}", [128, 4, D], BF16) for j in range(2)]
              hT = [k.sb(es3, f"hT_{j}", [128, 4, 512], BF16) for j in range(2)]
              sl = [k.sb(es3, f"sl_{j}", [128, 512], BF16) for j in range(2)]
              ph = Phase(k, "experts")
              s_wl = [ph.sig(), ph.sig()]
              s_hp = ph.sig()
              s_si = ph.sig()
              s_hd = ph.sig()
              s_yp = ph.sig()
              s_yd = ph.sig()
              s_we = ph.sig()
              bA = [banks[0], banks[1]]
              bB = [banks[2], banks[3]]
              bY = [banks[4], banks[5]]
              wl_vals = {}
              we_vals = {}

              def load_w(e_):
                  j = e_ % 2
                  if e_ >= 2:
                      ph.wait("gpsimd", s_we, we_vals[e_ - 2])
                  ph.dma("gpsimd", w1[j][:], w1_d[e_].rearrange("(c p) f -> p c f", p=128), s_wl[j])
                  ph.dma("gpsimd", w3[j][:], w3_d[e_].rearrange("(c p) f -> p c f", p=128), s_wl[j])
                  wl_vals[e_] = ph.dma("gpsimd", w2[j][:], w2_d[e_].rearrange("(c p) d -> p c d", p=128), s_wl[j])

              NER = int(os.environ.get("NER", NE))
              steps = [(e_, tb) for e_ in range(NER) for tb in range(4)]
              hd_vals = []
              hcount = 0
              ycount = 0
              yd_vals = []
              hstep_last = {}

              def emit_h(si):
                  nonlocal hcount
                  e_, tb = steps[si]
                  j = e_ % 2
                  hb = si % 2
                  if tb == 0:
                      ph.wait("tensor", s_wl[j], wl_vals[e_])
                  for fc in range(4):
                      jj = hcount % 2
                      if hcount >= 2:
                          ph.wait("tensor", s_hd, hd_vals[hcount - 2])
                      for c in range(KC):
                          ph.do("tensor", lambda e, o=bA[jj][:, :], l=w1[j][:, c, fc * 128:(fc + 1) * 128], r=XT[:, c, tb * 512:(tb + 1) * 512], st=(c == 0), sp=(c == KC - 1): e.matmul(o, lhsT=l, rhs=r, start=st, stop=sp))
                      for c in range(KC):
                          fn = lambda e, o=bB[jj][:, :], l=w3[j][:, c, fc * 128:(fc + 1) * 128], r=XT[:, c, tb * 512:(tb + 1) * 512], st=(c == 0), sp=(c == KC - 1): e.matmul(o, lhsT=l, rhs=r, start=st, stop=sp)
                          v = ph.do("tensor", fn, post=s_hp) if c == KC - 1 else ph.do("tensor", fn)
                      ph.wait("scalar", s_hp, v)
                      if hcount >= 2:
                          ph.wait("scalar", s_hd, hd_vals[hcount - 2])
                      vs = ph.do("scalar", lambda e, o=sl[jj][:, :], i=bA[jj][:, :]: e.activation(out=o, in_=i, func=AF.Silu), post=s_si)
                      ph.wait("vector", s_si, vs)
                      if fc == 0 and si >= 2:
                          ph.wait("vector", s_yp, ylast[si - 2])
                      vd = ph.do("vector", lambda e, o=hT[hb][:, fc, :], a=bB[jj][:, :], b=sl[jj][:, :]: e.tensor_tensor(out=o, in0=a, in1=b, op=ALU.mult), post=s_hd)
                      hd_vals.append(vd)
                      hcount += 1
                  hstep_last[si] = vd

              ylast = {}

              def emit_y(si):
                  nonlocal ycount
                  e_, tb = steps[si]
                  j = e_ % 2
                  hb = si % 2
                  ph.wait("tensor", s_hd, hstep_last[si])
                  for tt in range(4):
                      t = tb * 4 + tt
                      for half in range(2):
                          jj = ycount % 2
                          if ycount >= 2:
                              ph.wait("tensor", s_yd, yd_vals[ycount - 2])
                          for fc in range(4):
                              fn = lambda e, o=bY[jj][:, :], l=hT[hb][:, fc, tt * 128:(tt + 1) * 128], r=w2[j][:, fc, half * 512:(half + 1) * 512], st=(fc == 0), sp=(fc == 3): e.matmul(o, lhsT=l, rhs=r, start=st, stop=sp)
                              if fc == 3:
                                  if tt == 3 and half == 1 and tb == 3:
                                      v = ph.do("tensor", fn, post=s_yp)
                                  else:
                                      v = ph.do("tensor", fn, post=s_yp)
                              else:
                                  ph.do("tensor", fn)
                          ph.wait("vector", s_yp, v)
                          xs = X[:, t, half * 512:(half + 1) * 512]
                          vd = ph.do("vector", lambda e, o=xs, a=bY[jj][:, :], g=gates[:, t, e_:e_ + 1]: e.scalar_tensor_tensor(out=o, in0=a, scalar=g, in1=o, op0=ALU.mult, op1=ALU.add), post=s_yd)
                          yd_vals.append(vd)
                          ycount += 1
                  ylast[si] = v
                  if tb == 3:
                      we_vals[e_] = v

              s_we = s_yp
              load_w(0)
              load_w(1)
              nsteps = len(steps)
              emit_h(0)
              for si in range(nsteps):
                  if si + 1 < nsteps:
                      e_n, tb_n = steps[si + 1]
                      emit_h(si + 1)
                  emit_y(si)
                  e_, tb = steps[si]
                  if tb == 3 and e_ + 2 < NER:
                      load_w(e_ + 2)
              ph.emit()

    if 'noln' not in DBG:
        ln_phase(k, X, XT, ident, gb, stats, banks, dt_in["ln_ffn_g"][layer], dt_in["ln_ffn_b"][layer], do_transpose=not last)


def mixer_s5_pool(k, X, XT, ident, gb, stats, banks, dt_in):
    nc = k.nc
    PI = math.pi
    psall = k.psall
    with ExitStack() as es:
        HT = k.sb(es, "HT", [128, KC, S], BF16)
        CAT = XT
        YG = HT
        f32o = lambda name, shape: k.sb(es, name, shape, F32)
        LRE = f32o("LRE", [128, 16]); LIM = f32o("LIM", [128, 16]); LDT = f32o("LDT", [128, 16])
        BRE = f32o("BRE", [128, 16, 16]); BIM = f32o("BIM", [128, 16, 16])
        CRE = f32o("CRE", [128, 16, 16]); NCIM = f32o("NCIM", [128, 16, 16])
        DCOL = f32o("DCOL", [128, 4])
        gsig = Sig(k, NSEM - 1)
        with ExitStack() as es1:
            win = k.sb(es1, "win", [128, KC, D], BF16)
            ph = Phase(k, "inproj")
            lre_d, lim_d, ldt_d = dt_in["s5_lambda_re"], dt_in["s5_lambda_im"], dt_in["s5_log_dt"]
            dm = lambda o, i: ph.do("sync", lambda e, o=o, i=i: e.dma_start(out=o, in_=i, allow_slow_non_contiguous=True), post=gsig, n=16)
            dm(LRE[:], lre_d.rearrange("(gp g2) p -> (g2 p) gp", g2=2))
            dm(LIM[:], lim_d.rearrange("(gp g2) p -> (g2 p) gp", g2=2))
            ldv = ldt_d.rearrange("(gp g2) -> g2 gp", g2=2)
            for g2 in range(2):
                dm(LDT[g2 * 64:(g2 + 1) * 64, :], ldv[g2].partition_broadcast(64))
            dm(BRE[:], dt_in["s5_b_re"].rearrange("(gp g2) p h -> (g2 p) gp h", g2=2))
            dm(BIM[:], dt_in["s5_b_im"].rearrange("(gp g2) p h -> (g2 p) gp h", g2=2))
            crv = dt_in["s5_c_re"].rearrange("(gp g2) h p -> g2 p gp h", g2=2)
            civ = dt_in["s5_c_im"].rearrange("(gp g2) h p -> g2 p gp h", g2=2)
            for g2 in range(2):
                for gp in range(16):
                    dm(CRE[g2 * 64:(g2 + 1) * 64, gp, :], crv[g2, :, gp, :])
                    dm(NCIM[g2 * 64:(g2 + 1) * 64, gp, :], civ[g2, :, gp, :])
            dm(DCOL[:], dt_in["s5_d"].rearrange("(c p) -> p c", p=128))
            s_w = ph.sig()
            s_mm = ph.sig()
            s_ev = {"scalar": ph.sig(), "vector": ph.sig()}
            ph.dma("gpsimd", win[:], dt_in["ab_w_in"].rearrange("(c p) d -> p c d", p=128), s_w)
            ph.wait("tensor", s_w)
            evs = []
            n = 0
            for oc in range(KC):
                for tb in range(4):
                    jj = n % 2
                    if n >= 2:
                        ph.wait("tensor", evs[n - 2][0], evs[n - 2][1])
                    for c in range(KC):
                        fn = mm(banks[jj][:, :], win[:, c, oc * 128:(oc + 1) * 128], XT[:, c, tb * 512:(tb + 1) * 512], c == 0, c == KC - 1)
                        v = ph.do("tensor", fn, post=s_mm) if c == KC - 1 else ph.do("tensor", fn)
                    eng = "scalar" if n % 2 == 0 else "vector"
                    ph.wait(eng, s_mm, v)
                    o = HT[:, oc, tb * 512:(tb + 1) * 512]
                    if eng == "scalar":
                        fn = lambda e, o=o, i=banks[jj][:, :]: e.activation(out=o, in_=i, func=AF.Copy)
                    else:
                        fn = lambda e, o=o, i=banks[jj][:, :]: e.tensor_copy(out=o, in_=i)
                    evs.append((s_ev[eng], ph.do(eng, fn, post=s_ev[eng])))
                    n += 1
            ph.emit()

        with ExitStack() as es2:
            pw = k.sb(es2, "pw", [128, 4, 128], BF16)
            psc = k.sb(es2, "psc", [128, 4], F32)
            ici = k.sb(es2, "ici", [128, 16], I32)
            ic = k.sb(es2, "ic", [128, 16], F32)
            pA = k.sb(es2, "pA", [128, S], F32)
            pB = k.sb(es2, "pB", [128, S], F32)
            pP = k.sb(es2, "pP", [128, S], BF16)
            ph = Phase(k, "pool")
            sr = Serial(ph)
            sr.dma("gpsimd", pw[:], dt_in["pool_w"].rearrange("g c d -> c g d"))
            for gi in range(4):
                sr.dma("sync", psc[:, gi:gi + 1], dt_in["pool_scale"][gi * 128:(gi + 1) * 128].rearrange("(p o) -> p o", o=1))
            sr.op("gpsimd", lambda e: e.iota(ici[:], pattern=[[1, 16]], base=1, channel_multiplier=0))
            sr.op("vector", lambda e: e.tensor_copy(out=ic[:], in_=ici[:]))
            sr.op("vector", lambda e: e.reciprocal(out=ic[:], in_=ic[:]))
            for gi, w in enumerate((2, 4, 8, 16)):
                v = HT[:, 4 + gi, :]
                sr.op("vector", lambda e, v=v: e.tensor_copy(out=pA[:], in_=v))
                a, b = pA, pB
                kk = 1
                while kk < w:
                    sr.op("vector", lambda e, a=a, b=b, kk=kk: e.tensor_tensor(out=b[:, kk:S], in0=a[:, kk:S], in1=a[:, 0:S - kk], op=ALU.add))
                    sr.op("vector", lambda e, a=a, b=b, kk=kk: e.tensor_copy(out=b[:, 0:kk], in_=a[:, 0:kk]))
                    a, b = b, a
                    kk *= 2
                sr.op("vector", lambda e, a=a, v=v, w=w: e.scalar_tensor_tensor(out=pP[:], in0=a[:], scalar=1.0 / w, in1=v, op0=ALU.mult, op1=ALU.subtract))
                sr.op("vector", lambda e, a=a, b=b, w=w: e.tensor_tensor(out=b[:, 0:w - 1], in0=a[:, 0:w - 1], in1=ic[:, 0:w - 1], op=ALU.mult))
                sr.op("vector", lambda e, b=b, v=v, w=w: e.tensor_tensor(out=pP[:, 0:w - 1], in0=b[:, 0:w - 1], in1=v[:, 0:w - 1], op=ALU.subtract))
                for tb in range(4):
                    cs = slice(tb * 512, (tb + 1) * 512)
                    sr.op("tensor", mm(banks[0][:, :], pw[:, gi, :], pP[:, cs], True, True))
                    sr.op("scalar", lambda e, o=CAT[:, 4 + gi, cs], sc=psc[:, gi:gi + 1]: e.activation(out=o, in_=banks[0][:, :], func=AF.Identity, scale=sc))
            ph.emit()

        with ExitStack() as es3:
            f32t = lambda name, shape: k.sb(es3, name, shape, F32)
            BBRE = f32t("BBRE", [128, 16, 16]); BBIM = f32t("BBIM", [128, 16, 16])
            DT = f32t("DT", [128, 16]); MM_ = f32t("MM", [128, 16]); TH = f32t("TH", [128, 16])
            T0 = f32t("T0", [128, 16]); T1s = f32t("T1s", [128, 16]); T2s = f32t("T2s", [128, 16])
            CTH = f32t("CTH", [128, 16]); STH = f32t("STH", [128, 16])
            LBR = f32t("LBR", [128, 16]); LBI = f32t("LBI", [128, 16])
            FRE = f32t("FRE", [128, 16]); FIM = f32t("FIM", [128, 16]); DEN = f32t("DEN", [128, 16])
            OFF = f32t("OFF", [128, 16, 4])
            TVi = k.sb(es3, "TVi", [128, 512], I32)
            TVl = f32t("TVl", [128, 512])
            WG = k.sb(es3, "WG", [128, 4, 512], BF16)
            TMPB = f32t("TMPB", [128, 16, 16])
            E2 = f32t("E2", [128, 2, 128])
            BBT = k.sb(es3, "BBT", [128, 4, 3, 128], BF16)
            CP = k.sb(es3, "CP", [128, 4, 2, 128], BF16)
            TA = f32t("TA", [128, 512]); TC = f32t("TC", [128, 512])
            M1 = f32t("M1", [128, 2, 512]); M2 = f32t("M2", [128, 2, 512])
            WS = [f32t("WS0", [128, 2, 512]), f32t("WS1", [128, 2, 512])]
            XS = k.sb(es3, "XS", [128, 2, 512], BF16)
            MB = f32t("MB", [128, 512])
            PRE = f32t("PRE", [128, 512])
            CAR = f32t("CAR", [128, 2])

            ph = Phase(k, "s5")
            s_ldg = ph.sig()
            ph.dma("gpsimd", WG[:], dt_in["s5_w_glu"].rearrange("(c p) n -> p c n", p=128), s_ldg)

            sr = Serial(ph)
            sr.op("gpsimd", lambda e: e.iota(TVi[:], pattern=[[1, 512]], base=1, channel_multiplier=0))
            ph.wait("vector", gsig)
            ph.wait("vector", s_ldg)
            V_ = lambda fn: sr.op("vector", fn)
            A_ = lambda fn: sr.op("scalar", fn)
            tt = lambda o, a, b, op: (lambda e, o=o, a=a, b=b, op=op: e.tensor_tensor(out=o, in0=a, in1=b, op=op))
            ts = lambda o, a, s1, s2, op0, op1=None: (lambda e, o=o, a=a, s1=s1, s2=s2, op0=op0, op1=op1:
                                                    e.tensor_scalar(out=o, in0=a, scalar1=s1, scalar2=s2, op0=op0, op1=op1) if op1 is not None else
                                                    e.tensor_scalar(out=o, in0=a, scalar1=s1, scalar2=None, op0=op0))
            V_(lambda e: e.tensor_copy(out=TVl[:], in_=TVi[:]))
            V_(ts(NCIM[:], NCIM[:], -1.0, None, ALU.mult))
            A_(lambda e: e.activation(out=DT[:], in_=LDT[:], func=AF.Exp))
            V_(tt(T0[:], LRE[:], DT[:], ALU.mult))
            A_(lambda e: e.activation(out=MM_[:], in_=T0[:], func=AF.Exp))
            V_(tt(TH[:], LIM[:], DT[:], ALU.mult))
            halfpi = f32t("halfpi", [128, 1])
            V_(lambda e: e.memset(halfpi[:], 0.5 * PI))
            KIs = k.sb(es3, "KIs", [128, 64], I32)
            KFs = f32t("KFs", [128, 64])
            PS = 3.1415925

            def sincos(y, U, KI, KF, RC, SN, CS):
                V_(ts(U, y, 1.0 / (2.0 * PI), None, ALU.mult))
                V_(lambda e, KI=KI, U=U: e.tensor_copy(out=KI, in_=U))
                V_(lambda e, KI=KI, KF=KF: e.tensor_copy(out=KF, in_=KI))
                V_(lambda e, KF=KF, y=y: e.scalar_tensor_tensor(out=y, in0=KF, scalar=-2.0 * PI, in1=y, op0=ALU.mult, op1=ALU.add))
                V_(ts(y, y, -PS, PS, ALU.max, ALU.min))
                V_(ts(KF, y, 0.5 * PI, None, ALU.is_gt))
                V_(lambda e, KF=KF, y=y, RC=RC: e.scalar_tensor_tensor(out=RC, in0=KF, scalar=-2.0 * PI, in1=y, op0=ALU.mult, op1=ALU.add))
                A_(lambda e, SN=SN, y=y: e.activation(out=SN, in_=y, func=AF.Sin))
                A_(lambda e, CS=CS, RC=RC: e.activation(out=CS, in_=RC, func=AF.Sin, bias=halfpi[:, 0:1], scale=1.0))

            V_(lambda e: e.tensor_copy(out=T1s[:], in_=TH[:]))
            sincos(T1s[:], T2s[:], KIs[:, 0:16], KFs[:, 0:16], T2s[:], STH[:], CTH[:])
            V_(tt(LBR[:], MM_[:], CTH[:], ALU.mult))
            V_(tt(LBI[:], MM_[:], STH[:], ALU.mult))
            V_(tt(T0[:], LRE[:], LRE[:], ALU.mult))
            V_(tt(T1s[:], LIM[:], LIM[:], ALU.mult))
            V_(tt(DEN[:], T0[:], T1s[:], ALU.add))
            V_(lambda e: e.reciprocal(out=DEN[:], in_=DEN[:]))
            V_(ts(T2s[:], LBR[:], -1.0, None, ALU.add))
            V_(tt(T0[:], T2s[:], LRE[:], ALU.mult))
            V_(tt(T1s[:], LBI[:], LIM[:], ALU.mult))
            V_(tt(T0[:], T0[:], T1s[:], ALU.add))
            V_(tt(FRE[:], T0[:], DEN[:], ALU.mult))
            V_(tt(T0[:], LBI[:], LRE[:], ALU.mult))
            V_(tt(T1s[:], T2s[:], LIM[:], ALU.mult))
            V_(tt(T0[:], T0[:], T1s[:], ALU.subtract))
            V_(tt(FIM[:], T0[:], DEN[:], ALU.mult))
            fb = lambda F: F[:].unsqueeze(2).to_broadcast([128, 16, 16])
            V_(tt(BBRE[:], BRE[:], fb(FRE), ALU.mult))
            V_(tt(TMPB[:], BIM[:], fb(FIM), ALU.mult))
            V_(tt(BBRE[:], BBRE[:], TMPB[:], ALU.subtract))
            V_(tt(BBIM[:], BIM[:], fb(FRE), ALU.mult))
            V_(tt(TMPB[:], BRE[:], fb(FIM), ALU.mult))
            V_(tt(BBIM[:], BBIM[:], TMPB[:], ALU.add))
            OFFv = OFF[:].rearrange("p g t -> p (g t)")
            for tb in range(4):
                V_(ts(OFF[:, :, tb], TH[:], 512.0 * tb, None, ALU.mult))
            V_(ts(KFs[:, 0:64], OFFv, 1.0 / (2.0 * PI), None, ALU.mult))
            V_(lambda e: e.tensor_copy(out=KIs[:, 0:64], in_=KFs[:, 0:64]))
            V_(lambda e: e.tensor_copy(out=KFs[:, 0:64], in_=KIs[:, 0:64]))
            V_(lambda e: e.scalar_tensor_tensor(out=OFFv, in0=KFs[:, 0:64], scalar=-2.0 * PI, in1=OFFv, op0=ALU.mult, op1=ALU.add))
            KI = k.sb(es3, "KI", [128, 512], I32)
            KF = f32t("KF", [128, 512])

            spe = Stream(ph)
            XS2 = [XS, k.sb(es3, "XSb", [128, 2, 512], BF16)]
            S5C = {"n": 0, "bu_next": None, "m2": None, "cg": [None, None], "cgtb": [None] * 4, "ev": [None] * 4}
            bB = [banks[0], banks[1], banks[2]]
            A1 = psall[:, 0:2, :]
            A2 = psall[:, 1:3, :]
            bZ = banks[3]
            bY = [banks[4], banks[5], banks[6], banks[7]]
            for fc in range(4):
                sr.op("gpsimd", lambda e: e.memset(CP[:], 0.0))
                for pl in range(4):
                    gp = fc * 4 + pl
                    base = pl * 32
                    sr.op("gpsimd", lambda e: e.memset(E2[:], 0.0))
                    for g2 in range(2):
                        rs = slice(g2 * 64, (g2 + 1) * 64)
                        cs_ = slice(base + g2 * 16, base + g2 * 16 + 16)
                        V_(lambda e, rs=rs, cs_=cs_, gp=gp: e.tensor_copy(out=E2[rs, 0, cs_], in_=BBRE[rs, gp, :]))
                        V_(lambda e, rs=rs, cs_=cs_, gp=gp: e.tensor_copy(out=E2[rs, 1, cs_], in_=BBIM[rs, gp, :]))
                        V_(lambda e, rs=rs, cs_=cs_, gp=gp, pl=pl: e.tensor_copy(out=CP[rs, pl, 0, cs_], in_=CRE[rs, gp, :]))
                        V_(lambda e, rs=rs, cs_=cs_, gp=gp, pl=pl: e.tensor_copy(out=CP[rs, pl, 1, cs_], in_=NCIM[rs, gp, :]))
                    sr.group("tensor", [lambda e: e.transpose(bZ[:, 0:128], E2[:, 0, :], ident[:]),
                                        lambda e: e.transpose(bZ[:, 128:256], E2[:, 1, :], ident[:])])
                    A_(lambda e, pl=pl: e.activation(out=BBT[:, pl, 0:2, :], in_=bZ[:, 0:256].rearrange("p (v n) -> p v n", v=2), func=AF.Copy))
                    V_(lambda e, pl=pl: e.tensor_scalar(out=BBT[:, pl, 2, :], in0=bZ[:, 0:128], scalar1=-1.0, scalar2=None, op0=ALU.mult))
                for pl in range(4):
                    gp = fc * 4 + pl
                    V_(lambda e, gp=gp: e.tensor_copy(out=MB[:], in_=MM_[:, gp:gp + 1].to_broadcast([128, 512])))
                    V_(lambda e, gp=gp: e.tensor_scalar(out=TA[:], in0=TVl[:], scalar1=TH[:, gp:gp + 1], scalar2=None, op0=ALU.mult))
                    sincos(TA[:], TC[:], KI[:], KF[:], TC[:], TA[:], TC[:])
                    cb = TC[:].unsqueeze(1).to_broadcast([128, 2, 512])
                    sb_ = TA[:].unsqueeze(1).to_broadcast([128, 2, 512])
                    for tb in range(4):
                        cs = slice(tb * 512, (tb + 1) * 512)
                        stepi = S5C["n"]
                        xs = XS2[stepi % 2]
                        if S5C["bu_next"] is None:
                            S5C["bu_next"] = spe.emit("tensor", [mm(bB[v_][:, :], BBT[:, pl, v_, :], HT[:, fc, cs], True, True) for v_ in range(3)],
                                                      [Node(*sr.last), S5C["m2"]])
                        bu = S5C["bu_next"]
                        S5C["bu_next"] = None
                        ph.wait("vector", bu.sig, bu.val)
                        V_(tt(M1[:], A1, cb, ALU.mult))
                        m2 = V_(tt(M2[:], A2, sb_, ALU.mult))
                        S5C["m2"] = Node(*m2)
                        nxt = (pl, tb + 1) if tb < 3 else ((pl + 1, 0) if pl < 3 else None)
                        if nxt is not None:
                            ncs = slice(nxt[1] * 512, (nxt[1] + 1) * 512)
                            S5C["bu_next"] = spe.emit("tensor", [mm(bB[v_][:, :], BBT[:, nxt[0], v_, :], HT[:, fc, ncs], True, True) for v_ in range(3)], [S5C["m2"]])
                        V_(tt(M1[:], M1[:], M2[:], ALU.add))
                        for ri in range(2):
                            init = 0.0 if tb == 0 else CAR[:, ri:ri + 1]
                            V_(lambda e, ri=ri, init=init: e.tensor_tensor_scan(out=WS[0][:, ri, :], data0=MB[:], data1=M1[:, ri, :], initial=init, op0=ALU.mult, op1=ALU.add))
                        V_(tt(M1[:], WS[0][:], cb, ALU.mult))
                        V_(tt(M2[:], WS[0][:], sb_, ALU.mult))
                        if S5C["cg"][stepi % 2] is not None:
                            cgp = S5C["cg"][stepi % 2]
                            ph.wait("vector", cgp.sig, cgp.val)
                        V_(tt(xs[:, 0, :], M1[:, 0, :], M2[:, 1, :], ALU.subtract))
                        xn = V_(tt(xs[:, 1, :], M1[:, 1, :], M2[:, 0, :], ALU.add))
                        if tb < 3:
                            V_(tt(CAR[:, 0:1], M1[:, 0, 511:512], M2[:, 1, 511:512], ALU.subtract))
                            V_(tt(CAR[:, 1:2], M1[:, 1, 511:512], M2[:, 0, 511:512], ALU.add))
                        cgn = spe.emit("tensor", [mm(bY[tb][:, :], CP[:, pl, 0, :], xs[:, 0, :], pl == 0, False),
                                                  mm(bY[tb][:, :], CP[:, pl, 1, :], xs[:, 1, :], False, pl == 3)], [Node(*xn), S5C["ev"][tb]])
                        S5C["cg"][stepi % 2] = cgn
                        S5C["cgtb"][tb] = cgn
                        S5C["n"] += 1
                for tb in range(4):
                    cs = slice(tb * 512, (tb + 1) * 512)
                    ph.wait("vector", S5C["cgtb"][tb].sig, S5C["cgtb"][tb].val)
                    pe_ = V_(lambda e, cs=cs, tb=tb, fc=fc: e.scalar_tensor_tensor(out=PRE[:], in0=HT[:, fc, cs], scalar=DCOL[:, fc:fc + 1], in1=bY[tb][:, :], op0=ALU.mult, op1=ALU.add))
                    S5C["ev"][tb] = Node(*pe_)
                    A_(lambda e, cs=cs, fc=fc: e.activation(out=YG[:, 4 + fc, cs], in_=PRE[:], func=AF.Gelu_apprx_tanh))
            for oc in range(4):
                for tb in range(4):
                    cs = slice(tb * 512, (tb + 1) * 512)
                    sr.group("tensor", [mm(bZ[:, :], WG[:, c, oc * 128:(oc + 1) * 128], YG[:, 4 + c, cs], c == 0, c == 3) for c in range(4)])
                    A_(lambda e: e.activation(out=PRE[:], in_=bZ[:, :], func=AF.Sigmoid))
                    V_(lambda e, oc=oc, cs=cs: e.tensor_tensor(out=CAT[:, oc, cs], in0=YG[:, 4 + oc, cs], in1=PRE[:], op=ALU.mult))
            ph.emit()

        outproj_residual_ln(k, X, XT, CAT, dt_in["ab_w_out"], dt_in["ln_mix_g"][0], dt_in["ln_mix_b"][0], ident, gb, stats, banks)


class Serial:
    def __init__(self, ph):
        self.ph = ph
        self.sig = ph.sig()
        self.dsig = ph.sig()
        self.last = None

    def _wait(self, eng):
        if self.last is not None:
            self.ph.wait(eng, self.last[0], self.last[1])

    def op(self, eng, fn):
        self._wait(eng)
        self.last = (self.sig, self.ph.do(eng, fn, post=self.sig))
        return self.last

    def group(self, eng, fns):
        self._wait(eng)
        for f in fns[:-1]:
            self.ph.do(eng, f)
        self.last = (self.sig, self.ph.do(eng, fns[-1], post=self.sig))
        return self.last

    def dma(self, eng, out, in_):
        self._wait(eng)
        self.last = (self.dsig, self.ph.dma(eng, out, in_, self.dsig))
        return self.last


def mm(o, l, r, st, sp):
    return lambda e, o=o, l=l, r=r, st=st, sp=sp: e.matmul(o, lhsT=l, rhs=r, start=st, stop=sp)


def outproj_residual_ln(k, X, XT, CT, w_d, g_d, b_d, ident, gb, stats, banks):
    with ExitStack() as es:
        wo = k.sb(es, "wo", [128, KC, D], BF16)
        ph = Phase(k, "outproj")
        s_w = ph.sig()
        s_mm = ph.sig()
        s_dv = ph.sig()
        ph.dma("gpsimd", wo[:], w_d.rearrange("(c p) d -> p c d", p=128), s_w)
        ph.wait("tensor", s_w)
        dvs = []
        n = 0
        for t in range(NT):
            for hb in range(2):
                jj = n % 2
                if n >= 2:
                    ph.wait("tensor", s_dv, dvs[n - 2])
                for c in range(KC):
                    fn = mm(banks[jj][:, :], CT[:, c, t * 128:(t + 1) * 128], wo[:, c, hb * 512:(hb + 1) * 512], c == 0, c == KC - 1)
                    v = ph.do("tensor", fn, post=s_mm) if c == KC - 1 else ph.do("tensor", fn)
                ph.wait("vector", s_mm, v)
                xs = X[:, t, hb * 512:(hb + 1) * 512]
                dvs.append(ph.do("vector", lambda e, o=xs, b=banks[jj][:, :]: e.scalar_tensor_tensor(out=o, in0=o, scalar=ALPHA, in1=b, op0=ALU.mult, op1=ALU.add), post=s_dv))
                n += 1
        ph.emit()
    ln_phase(k, X, XT, ident, gb, stats, banks, g_d, b_d, do_transpose=True)


class Node:
    __slots__ = ("sig", "val")

    def __init__(self, sig, val):
        self.sig = sig
        self.val = val


class Stream:
    def __init__(self, ph):
        self.ph = ph
        self.sigs = {}

    def emit(self, eng, fns, deps=()):
        if not isinstance(fns, (list, tuple)):
            fns = [fns]
        if eng not in self.sigs:
            self.sigs[eng] = self.ph.sig()
        sig = self.sigs[eng]
        for d in deps:
            if d is None:
                continue
            assert d.val <= d.sig.val, "dependency on a not-yet-emitted op"
            self.ph.wait(eng, d.sig, d.val)
        for f in fns[:-1]:
            self.ph.do(eng, f)
        v = self.ph.do(eng, fns[-1], post=sig)
        return Node(sig, v)


def mixer_attention(k, X, XT, ident, gb, stats, banks, dt_in):
    nc = k.nc
    wqkv = dt_in["sb_w_qkv"].rearrange("(c p) n -> p c n", p=128)
    with ExitStack() as eso:
      OT = k.sb(eso, "OT", [128, KC, S], BF16)
      with ExitStack() as es:
        tri = k.sb(es, "tri", [128, 128], BF16)
        ones = k.sb(es, "ones", [128, 128], BF16)
        mlt = k.sb(es, "mlt", [128, 128], BF16)
        QTn = k.sb(es, "QTn", [128, S], BF16)
        KTh = [k.sb(es, f"KTh{h}", [128, S], BF16) for h in range(2)]
        Vh = [k.sb(es, f"Vh{h}", [128, NT, 128], BF16) for h in range(2)]
        wq = k.sb(es, "wq", [128, 3, KC, 128], BF16)
        NCH = 4
        Eb = [k.sb(es, f"Eb{c}", [128, 512], F32) for c in range(NCH)]
        SP = [k.sb(es, f"SP{c}", [128, 512], BF16) for c in range(NCH)]
        SPs = [k.sb(es, f"SPs{c}", [128, 512], BF16) for c in range(NCH)]
        Wt = [k.sb(es, f"Wt{c}", [128, 512], BF16) for c in range(NCH)]
        bz = [banks[0], banks[1], banks[2], banks[3]]
        bo = [banks[4], banks[5], banks[6], banks[7]]

        ph = Phase(k, "attn_consts")
        sc_ = Stream(ph)
        n0 = sc_.emit("gpsimd", lambda e: e.memset(ones[:], 1.0))
        n1 = sc_.emit("gpsimd", lambda e: e.affine_select(out=tri[:], in_=ones[:], pattern=[[-1, 128]], compare_op=ALU.is_ge,
                                                           fill=0.0, base=0, channel_multiplier=1), [n0])
        sc_.emit("gpsimd", lambda e: e.affine_select(out=mlt[:], in_=ones[:], pattern=[[1, 128]], compare_op=ALU.is_gt,
                                                      fill=0.0, base=0, channel_multiplier=-1), [n1])
        sc_.emit("vector", lambda e: e.memset(KTh[0][64:128, :], 0.0))
        sc_.emit("vector", lambda e: e.memset(KTh[1][0:64, :], 0.0))
        sc_.emit("vector", lambda e: e.memset(Vh[0][:, :, 64:128], 0.0))
        sc_.emit("vector", lambda e: e.memset(Vh[1][:, :, 0:64], 0.0))
        ph.emit()

        for hc in range(int(os.environ.get('NHC', KC))):
            ph = Phase(k, f"attn{hc}")
            sp_ = Stream(ph)
            sd = ph.sig()
            for i3 in range(3):
                vd = ph.dma("gpsimd", wq[:, i3, :, :], wqkv[:, :, i3 * D + hc * 128:i3 * D + (hc + 1) * 128], sd)
            wnode = Node(sd, vd)
            ev = [None] * 4
            pj = []
            n = 0
            for tb in range(4):
                cs = slice(tb * 512, (tb + 1) * 512)
                for which in range(2):
                    bk = banks[n % 4]
                    m = sp_.emit("tensor", [mm(bk[:, :], wq[:, which, c, :], XT[:, c, cs], c == 0, c == KC - 1) for c in range(KC)], [wnode, ev[n % 4]])
                    if which == 0:
                        e2 = sp_.emit("vector", lambda e, o=QTn[:, cs], bk=bk: e.tensor_scalar(out=o, in0=bk[:, :], scalar1=-0.125, scalar2=None, op0=ALU.mult), [m])
                    else:
                        e1 = sp_.emit("vector", lambda e, o=KTh[0][0:64, cs], bk=bk: e.tensor_copy(out=o, in_=bk[0:64, :]), [m])
                        e2 = sp_.emit("vector", lambda e, o=KTh[1][64:128, cs], bk=bk: e.tensor_copy(out=o, in_=bk[64:128, :]), [m, e1])
                        pj.append(e1)
                    ev[n % 4] = e2
                    pj.append(e2)
                    n += 1
            for q4 in range(4):
                bk = banks[n % 4]
                fns = []
                for tt in range(4):
                    t = q4 * 4 + tt
                    fns += [mm(bk[:, tt * 128:(tt + 1) * 128], XT[:, c, t * 128:(t + 1) * 128], wq[:, 2, c, :], c == 0, c == KC - 1) for c in range(KC)]
                m = sp_.emit("tensor", fns, [wnode, ev[n % 4]])
                bv = bk[:, :].rearrange("p (t n) -> p t n", t=4)
                e0 = sp_.emit("vector", lambda e, o=Vh[0][:, q4 * 4:(q4 + 1) * 4, 0:64], i=bv[:, :, 0:64]: e.tensor_copy(out=o, in_=i), [m])
                e1 = sp_.emit("vector", lambda e, o=Vh[1][:, q4 * 4:(q4 + 1) * 4, 64:128], i=bv[:, :, 64:128]: e.tensor_copy(out=o, in_=i), [m, e0])
                ev[n % 4] = e1
                pj += [e0, e1]
                n += 1
            projdone = pj

            NQC = int(os.environ.get('NQC', 4))
            batches = [[(0, 3), (1, 3), (0, 2), (1, 2)], [(0, 1), (1, 1), (0, 0), (1, 0)]]
            streams = [Stream(ph) for _ in range(NCH)]
            prev_oev = [None] * NCH
            first_batch = True
            for batch in batches:
                batch = [(h, qc) for (h, qc) in batch if qc < NQC]
                st = [dict() for _ in range(NCH)]
                maxlen = max([4 * qc + 4 for (_, qc) in batch] + [0])
                for s_ in range(maxlen):
                    act = [(c, h, qc) for c, (h, qc) in enumerate(batch) if s_ < 4 * qc + 4]
                    geo = {}
                    for c, h, qc in act:
                        jmax = 4 * qc + 3
                        j = jmax - s_
                        off = max(0, j - 4 * qc) * 128
                        geo[c] = (j, jmax, off, 512 - off, slice(qc * 512 + off, (qc + 1) * 512), slice(j * 128, (j + 1) * 128), j >= 4 * qc)
                    for c, h, qc in act:
                        if s_ == 0:
                            st[c]["rst"] = streams[c].emit("vector", [lambda e, o=SPs[c][:, :]: e.memset(o, 0.0),
                                                                    lambda e, o=Wt[c][:, 0:384]: e.memset(o, 0.0)], [prev_oev[c]] + (projdone if first_batch else []))
                    for c, h, qc in act:
                        j, jmax, off, N, qs, ks, diag = geo[c]
                        st[c]["zm"] = streams[c].emit("tensor", mm(bz[c][:, 0:N], KTh[h][:, ks], QTn[:, qs], True, False),
                                                      [st[c].get("ew"), st[c]["rst"]] + (projdone if first_batch and s_ == 0 else []))
                    for c, h, qc in act:
                        j, jmax, off, N, qs, ks, diag = geo[c]
                        st[c]["ex"] = streams[c].emit("scalar", lambda e, o=Eb[c][:, 0:N], z=bz[c][:, 0:N]: e.activation(out=o, in_=z, func=AF.Exp, scale=-1.0),
                                                      [st[c]["zm"], st[c].get("ln")])
                    for c, h, qc in act:
                        j, jmax, off, N, qs, ks, diag = geo[c]
                        st[c]["ln"] = streams[c].emit("scalar", lambda e, o=SP[c][:, 0:N], z=Eb[c][:, 0:N]: e.activation(out=o, in_=z, func=AF.Ln, bias=1.0),
                                                      [st[c]["ex"], st[c].get("cg"), st[c].get("sum")])
                        st[c]["al"] = st[c]["ln"]
                    for c, h, qc in act:
                        j, jmax, off, N, qs, ks, diag = geo[c]
                        if diag:
                            st[c]["al"] = streams[c].emit("vector", lambda e, o=SP[c][:, 0:128]: e.tensor_tensor(out=o, in0=o, in1=mlt[:, :], op=ALU.mult), [st[c]["ln"]])
                    for c, h, qc in act:
                        j, jmax, off, N, qs, ks, diag = geo[c]
                        fns = [mm(bz[c][:, 0:N], tri[:, :], SP[c][:, 0:N], False, j == jmax)]
                        if j < jmax:
                            fns.append(mm(bz[c][:, 0:N], ones[:, :], SPs[c][:, off:512], False, True))
                        st[c]["cg"] = streams[c].emit("tensor", fns, [st[c]["al"], st[c].get("sum"), st[c]["rst"], st[c]["ex"]])
                    for c, h, qc in act:
                        j, jmax, off, N, qs, ks, diag = geo[c]
                        st[c]["ew"] = streams[c].emit("scalar", lambda e, o=Wt[c][:, off:512], z=bz[c][:, 0:N]: e.activation(out=o, in_=z, func=AF.Exp, scale=-1.0),
                                                      [st[c]["cg"], st[c].get("pv"), st[c]["rst"]])
                        st[c]["wl"] = st[c]["ew"]
                    for c, h, qc in act:
                        j, jmax, off, N, qs, ks, diag = geo[c]
                        if diag:
                            st[c]["wl"] = streams[c].emit("vector", lambda e, o=Wt[c][:, off:off + 128]: e.tensor_tensor(out=o, in0=o, in1=mlt[:, :], op=ALU.mult), [st[c]["ew"]])
                    for c, h, qc in act:
                        j, jmax, off, N, qs, ks, diag = geo[c]
                        if j > 0:
                            st[c]["sum"] = streams[c].emit("vector", lambda e, o=SPs[c][:, off:512], a_=SP[c][:, 0:N]: e.tensor_tensor(out=o, in0=o, in1=a_, op=ALU.add),
                                                           [st[c]["cg"], st[c]["al"], st[c]["rst"]])
                    for c, h, qc in act:
                        j, jmax, off, N, qs, ks, diag = geo[c]
                        st[c]["pv"] = streams[c].emit("tensor", mm(bo[c][:, 0:512], Vh[h][:, j, :], Wt[c][:, 0:512], j == jmax, j == 0),
                                                      [st[c]["wl"], prev_oev[c] if j == jmax else None])
                    for c, h, qc in act:
                        j, jmax, off, N, qs, ks, diag = geo[c]
                        if j == 0:
                            o_ = OT[h * 64:(h + 1) * 64, hc, qc * 512:(qc + 1) * 512]
                            i_ = bo[c][h * 64:(h + 1) * 64, :]
                            prev_oev[c] = streams[c].emit("vector", lambda e, o_=o_, i_=i_: e.tensor_copy(out=o_, in_=i_), [st[c]["pv"]])
                first_batch = False
            ph.emit()
      outproj_residual_ln(k, X, XT, OT, dt_in["sb_w_out"], dt_in["ln_mix_g"][1], dt_in["ln_mix_b"][1], ident, gb, stats, banks)


_NAMES = ["x", "p", "ab_w_in", "s5_lambda_re", "s5_lambda_im", "s5_log_dt", "s5_b_re", "s5_b_im", "s5_c_re", "s5_c_im",
          "s5_d", "s5_w_glu", "pool_w", "pool_scale", "ab_w_out", "sb_w_qkv", "sb_w_out", "ln_mix_g", "ln_mix_b",
          "ln_ffn_g", "ln_ffn_b", "router_w", "router_bias", "moe_w1", "moe_w3", "moe_w2", "ple_w_proj", "ple_w_gate"]


def make_in_maps(inputs, cores):
    f = lambda a: np.ascontiguousarray(np.asarray(a, dtype=np.float32))
    shared = {
        "ab_w_in": f(inputs["ab_w_in"])[0],
        "s5_lambda_re": f(inputs["s5_lambda_re"])[0],
        "s5_lambda_im": f(inputs["s5_lambda_im"])[0],
        "s5_log_dt": f(inputs["s5_log_dt"])[0],
        "s5_b_re": f(inputs["s5_b_re"])[0],
        "s5_b_im": f(inputs["s5_b_im"])[0],
        "s5_c_re": f(inputs["s5_c_re"])[0],
        "s5_c_im": f(inputs["s5_c_im"])[0],
        "s5_d": f(inputs["s5_d"])[0].reshape(512),
        "s5_w_glu": f(inputs["s5_w_glu"])[0],
        "pool_w": f(inputs["pool_w"])[0],
        "pool_scale": f(inputs["pool_scale"])[0],
        "ab_w_out": f(inputs["ab_w_out"])[0],
        "sb_w_qkv": f(inputs["sb_w_qkv"])[0],
        "sb_w_out": f(inputs["sb_w_out"])[0],
        "ln_mix_g": f(inputs["ln_mix_g"]),
        "ln_mix_b": f(inputs["ln_mix_b"]),
        "ln_ffn_g": f(inputs["ln_ffn_g"]),
        "ln_ffn_b": f(inputs["ln_ffn_b"]),
        "router_w": f(inputs["router_w"]),
        "router_bias": f(inputs["router_bias"]),
        "moe_w1": f(inputs["moe_w1"]),
        "moe_w3": f(inputs["moe_w3"]),
        "moe_w2": f(inputs["moe_w2"]),
        "ple_w_proj": f(inputs["ple_w_proj"]),
        "ple_w_gate": f(inputs["ple_w_gate"]),
    }
    x = f(inputs["x"])
    p = f(inputs["p"])
    maps = []
    for c in cores:
        m = dict(shared)
        m["x"] = np.ascontiguousarray(x[c])
        m["p"] = np.ascontiguousarray(p[:, c])
        maps.append(m)
    return maps


def kernel(**inputs):
    nc = bass.Bass("TRN2", target_bir_lowering=False)
    build_program(nc)
    cores = list(range(8))
    in_maps = make_in_maps(inputs, cores)
    res = run_bass_kernel_spmd(nc, in_maps, core_ids=cores)
    out = np.stack([np.asarray(r["y"], dtype=np.float32) for r in res.results], axis=0)
    return out
```

```python
import math
import os
DBG = os.environ.get('KDBG', '')
from contextlib import ExitStack

import numpy as np
import concourse.bass as bass
import concourse.mybir as mybir
from concourse.bass_utils import run_bass_kernel_spmd

F32 = mybir.dt.float32
BF16 = mybir.dt.bfloat16
I32 = mybir.dt.int32
AF = mybir.ActivationFunctionType
ALU = mybir.AluOpType
AX = mybir.AxisListType

S = 2048
D = 1024
NT = 16
KC = 8
NE = 16
DE = 512
PLE = 256
ALPHA = 4.0 ** 0.25
LN_EPS = 1e-5
ENGS = ("tensor", "vector", "scalar", "gpsimd", "sync")
NSEM = 48


class Sig:
    def __init__(self, k, idx):
        self.k = k
        self.idx = idx

    @property
    def sem(self):
        return self.k.sems[self.idx]

    @property
    def val(self):
        return self.k.counts[self.idx]

    def post(self, n=1):
        self.k.counts[self.idx] += n
        return self.k.counts[self.idx]


class Phase:
    def __init__(self, k, name):
        self.k = k
        self.name = name
        self.ops = {e: [] for e in ENGS}
        self.selfsig = {}
        self.waited = {}
        k.next_sem = 0

    def sig(self):
        i = self.k.next_sem
        self.k.next_sem += 1
        assert i < NSEM, "out of semaphores"
        return Sig(self.k, i)

    def do(self, eng, fn, post=None, n=None):
        if post is not None:
            if n is None:
                n = 1
            v = post.post(n)
            sem = post.sem
            self.ops[eng].append(lambda e, fn=fn, sem=sem, n=n: fn(e).then_inc(sem, n))
            return v
        self.ops[eng].append(fn)
        return None

    def dos(self, eng, fn):
        if eng not in self.selfsig:
            self.selfsig[eng] = self.sig()
        sg = self.selfsig[eng]
        v = self.do(eng, fn, post=sg)
        self.wait(eng, sg, v)
        return v

    def dma(self, eng, out, in_, post):
        return self.do(eng, lambda e, out=out, in_=in_: e.dma_start(out=out, in_=in_), post=post, n=16)

    def wait(self, eng, sig, v=None):
        if v is None:
            v = sig.val
        if v <= 0:
            return
        key = (eng, sig.idx)
        if self.waited.get(key, 0) >= v:
            return
        self.waited[key] = v
        sem = sig.sem
        self.ops[eng].append(lambda e, sem=sem, v=v: e.wait_ge(sem, v))

    def emit(self):
        nc = self.k.nc
        with nc.Block() as b:
            for eng in ENGS:
                lst = self.ops[eng]
                if not lst:
                    continue

                def body(e, lst=lst):
                    for f in lst:
                        f(e)

                getattr(b, eng)(body)


class K:
    def __init__(self, nc, es):
        self.nc = nc
        self.es = es
        self.sems = [es.enter_context(nc.semaphore(f"sm{i}")) for i in range(NSEM)]
        self.counts = [0] * NSEM
        self.next_sem = 0

    def sb(self, es, name, shape, dt):
        self.uid = getattr(self, "uid", 0) + 1
        return es.enter_context(self.nc.sbuf_tensor(f"{name}_u{self.uid}", list(shape), dt))


def transpose_tiles(k, ph, X, XT, tiles, ready, banks, ident, done_sig=None):
    psf_e = {"scalar": ph.sig(), "vector": ph.sig()}
    pst = ph.sig()
    evs = []
    for n, t in enumerate(tiles):
        j = n % 2
        if t in ready:
            ph.wait("tensor", ready[t][0], ready[t][1])
        if n >= 2:
            ph.wait("tensor", evs[n - 2][0], evs[n - 2][1])
        for c in range(KC):
            bank = banks[2 * j + c // 4]
            out = bank[:, (c % 4) * 128:(c % 4 + 1) * 128]
            fn = lambda e, out=out, in_=X[:, t, c * 128:(c + 1) * 128]: e.transpose(out, in_, ident[:])
            if c == KC - 1:
                v = ph.do("tensor", fn, post=pst)
            else:
                ph.do("tensor", fn)
        eng = "scalar" if n % 2 == 0 else "vector"
        ph.wait(eng, pst, v)
        for hb in range(2):
            bank = banks[2 * j + hb]
            out = XT[:, hb * 4:(hb + 1) * 4, t * 128:(t + 1) * 128]
            in_ = bank[:, :].rearrange("p (c n) -> p c n", c=4)
            if eng == "scalar":
                fn = lambda e, out=out, in_=in_: e.activation(out=out, in_=in_, func=AF.Copy)
            else:
                fn = lambda e, out=out, in_=in_: e.tensor_copy(out=out, in_=in_)
            if hb == 1:
                evs.append((psf_e[eng], ph.do(eng, fn, post=psf_e[eng])))
            else:
                ph.do(eng, fn)
    return psf_e, evs


def ln_tiles(k, ph, X, tiles, ready, gb, stats, eps_t):
    s_st = ph.sig()
    s_sq = ph.sig()
    s_act = ph.sig()
    s_pool = ph.sig()
    s_dvgb = ph.sig()
    out_ready = {}
    act_vals = {}
    sq_vals = {}
    deferred = []
    nt = len(tiles)

    def stage_a(n):
        t = tiles[n]
        if t in ready:
            ph.wait("vector", ready[t][0], ready[t][1])
        if n >= 4:
            ph.wait("vector", s_act, act_vals[n - 4])
        st = stats[:, n % 4, :]
        for hb in range(2):
            ph.dos("vector", lambda e, o=st[:, hb * 6:(hb + 1) * 6], i=X[:, t, hb * 512:(hb + 1) * 512]: e.bn_stats(out=o, in_=i))
        va = ph.do("vector", lambda e, o=st[:, 12:14], i=st[:, 0:12]: e.bn_aggr(out=o, in_=i), post=s_st)
        ph.wait("scalar", s_st, va)
        sq_vals[n] = ph.do("scalar", lambda e, o=st[:, 14:15], i=st[:, 13:14]: e.activation(out=o, in_=i, func=AF.Sqrt, bias=eps_t[:, 0:1], scale=1.0), post=s_sq)

    def stage_b(n):
        t = tiles[n]
        st = stats[:, n % 4, :]
        rstd = st[:, 14:15]
        nmr = st[:, 15:16]
        ph.wait("vector", s_sq, sq_vals[n])
        ph.dos("vector", lambda e, o=rstd: e.reciprocal(out=o, in_=o))
        v = ph.do("vector", lambda e, o=nmr, i=st[:, 12:13], r=rstd: e.scalar_tensor_tensor(out=o, in0=i, scalar=-1.0, in1=r, op0=ALU.mult, op1=ALU.mult), post=s_st)
        ph.wait("scalar", s_st, v)
        v = ph.do("scalar", lambda e, o=X[:, t, :], r=rstd, b=nmr: e.activation(out=o, in_=o, func=AF.Identity, bias=b, scale=r), post=s_act)
        act_vals[n] = v
        if n % 2 == 0:
            ph.wait("gpsimd", s_act, v)
            ph.dos("gpsimd", lambda e, o=X[:, t, :], g=gb[:, 0, :]: e.tensor_tensor(out=o, in0=o, in1=g, op=ALU.mult))
            v2 = ph.do("gpsimd", lambda e, o=X[:, t, :], g=gb[:, 1, :]: e.tensor_tensor(out=o, in0=o, in1=g, op=ALU.add), post=s_pool)
            out_ready[t] = (s_pool, v2)
        else:
            def gbops(t=t, v=v):
                ph.wait("vector", s_act, v)
                ph.dos("vector", lambda e, o=X[:, t, :], g=gb[:, 0, :]: e.tensor_tensor(out=o, in0=o, in1=g, op=ALU.mult))
                v2 = ph.do("vector", lambda e, o=X[:, t, :], g=gb[:, 1, :]: e.tensor_tensor(out=o, in0=o, in1=g, op=ALU.add), post=s_dvgb)
                out_ready[t] = (s_dvgb, v2)
            deferred.append(gbops)

    stage_a(0)
    for n in range(nt):
        if n + 1 < nt:
            stage_a(n + 1)
        stage_b(n)
        if n % 2 == 0 and deferred:
            deferred.pop(0)()
    while deferred:
        deferred.pop(0)()
    return out_ready, s_act


def bcast_row(dram_vec_ap, n):
    return dram_vec_ap.partition_broadcast(128)


def build_program(nc, plan=("mix0", "ffn0", "mix1", "ffn1")):
    dt_in = {}

    def din(name, shape):
        dt_in[name] = nc.dram_tensor(name, list(shape), F32, kind="ExternalInput").ap()
        return dt_in[name]

    x_d = din("x", [S, D])
    p_d = din("p", [2, S, PLE])
    ab_w_in = din("ab_w_in", [D, D])
    s5_lre = din("s5_lambda_re", [32, 64])
    s5_lim = din("s5_lambda_im", [32, 64])
    s5_ldt = din("s5_log_dt", [32])
    s5_bre = din("s5_b_re", [32, 64, 16])
    s5_bim = din("s5_b_im", [32, 64, 16])
    s5_cre = din("s5_c_re", [32, 16, 64])
    s5_cim = din("s5_c_im", [32, 16, 64])
    s5_d = din("s5_d", [512])
    s5_wglu = din("s5_w_glu", [512, 512])
    pool_w = din("pool_w", [4, 128, 128])
    pool_scale = din("pool_scale", [512])
    ab_w_out = din("ab_w_out", [D, D])
    sb_w_qkv = din("sb_w_qkv", [D, 3 * D])
    sb_w_out = din("sb_w_out", [D, D])
    ln_mix_g = din("ln_mix_g", [2, D])
    ln_mix_b = din("ln_mix_b", [2, D])
    ln_ffn_g = din("ln_ffn_g", [2, D])
    ln_ffn_b = din("ln_ffn_b", [2, D])
    router_w = din("router_w", [D, NE])
    router_bias = din("router_bias", [NE])
    moe_w1 = din("moe_w1", [2, NE, D, DE])
    moe_w3 = din("moe_w3", [2, NE, D, DE])
    moe_w2 = din("moe_w2", [2, NE, DE, D])
    ple_w_proj = din("ple_w_proj", [2, PLE, D])
    ple_w_gate = din("ple_w_gate", [2, D, D])
    y_d = nc.dram_tensor("y", [S, D], F32, kind="ExternalOutput").ap()

    with ExitStack() as es:
        k = K(nc, es)
        X = k.sb(es, "X", [128, NT, D], F32)
        XT = k.sb(es, "XT", [128, KC, S], BF16)
        ident = k.sb(es, "ident", [128, 128], F32)
        gb = k.sb(es, "gb", [128, 2, D], F32)
        stats = k.sb(es, "stats", [128, 4, 16], F32)
        k.eps_t = k.sb(es, "eps_t", [128, 1], F32)
        psall = es.enter_context(nc.psum_tensor("psall", [128, 8, 512], F32))
        k.psall = psall
        banks = [psall[:, i, :] for i in range(8)]

        ph = Phase(k, "load")
        s_ld = ph.sig()
        s_id = ph.sig()
        ph.dos("gpsimd", lambda e: e.memset(ident[:], 0.0))
        ph.dos("gpsimd", lambda e: e.memset(k.eps_t[:], LN_EPS))
        ph.do("gpsimd", lambda e: e.affine_select(out=ident[:], in_=ident[:], pattern=[[-1, 128]], compare_op=ALU.not_equal,
                                                   fill=1.0, base=0, channel_multiplier=1), post=s_id)
        ready = {}
        xv = x_d.rearrange("(t p) d -> p t d", p=128)
        for q in range(4):
            sq = ph.sig()
            for t in range(q * 4, q * 4 + 4):
                v = ph.dma("sync", X[:, t, :], xv[:, t, :], sq)
            for t in range(q * 4, q * 4 + 4):
                ready[t] = (sq, v)
        ph.wait("tensor", s_id)
        transpose_tiles(k, ph, X, XT, list(range(NT)), ready, banks[0:4], ident)
        ph.emit()
        if 'dumpxt' in DBG:
            dbg_d = nc.dram_tensor("dbg", [128, KC * S], F32, kind="ExternalOutput").ap()
            ph = Phase(k, "dump")
            sd = ph.sig()
            for c in range(KC):
                ph.dma("gpsimd", dbg_d[:, c * S:(c + 1) * S], XT[:, c, :], sd)
            ph.wait("gpsimd", sd)
            ph.emit()

        for step in plan:
            layer = int(step[-1])
            if step.startswith("mix"):
                if layer == 0:
                    mixer_s5_pool(k, X, XT, ident, gb, stats, banks, dt_in)
                else:
                    mixer_attention(k, X, XT, ident, gb, stats, banks, dt_in)
            else:
                ffn_layer(k, layer, X, XT, ident, gb, stats, banks, dt_in, last=(step == plan[-1]))

        ph = Phase(k, "store")
        s_st = ph.sig()
        yv = y_d.rearrange("(t p) d -> p t d", p=128)
        for t in range(NT):
            ph.dma("sync", yv[:, t, :], X[:, t, :], s_st)
        ph.wait("sync", s_st)
        ph.emit()
    return nc


def load_gb(ph, gb, g_d, b_d, sig):
    ph.dma("sync", gb[:, 0, :], g_d.partition_broadcast(128), sig)
    return ph.dma("sync", gb[:, 1, :], b_d.partition_broadcast(128), sig)


def ln_phase(k, X, XT, ident, gb, stats, banks, g_d, b_d, do_transpose):
    ph = Phase(k, "ln")
    s_gb = ph.sig()
    load_gb(ph, gb, g_d, b_d, s_gb)
    ph.wait("gpsimd", s_gb)
    ph.wait("vector", s_gb)
    ready, _ = ln_tiles(k, ph, X, list(range(NT)), {}, gb, stats, k.eps_t)
    ph.emit()
    if do_transpose:
        ph = Phase(k, "lnT")
        transpose_tiles(k, ph, X, XT, list(range(NT)), {}, banks[0:4], ident)
        ph.emit()


def ffn_layer(k, layer, X, XT, ident, gb, stats, banks, dt_in, last):
    nc = k.nc
    p_d = dt_in["p"][layer]
    wp_d = dt_in["ple_w_proj"][layer]
    wg_d = dt_in["ple_w_gate"][layer]
    wr_d = dt_in["router_w"]
    rb_d = dt_in["router_bias"]
    w1_d = dt_in["moe_w1"][layer]
    w3_d = dt_in["moe_w3"][layer]
    w2_d = dt_in["moe_w2"][layer]

    with ExitStack() as es:
        gates = k.sb(es, "gates", [128, NT, NE], F32)
        with ExitStack() as es1:
            wg = k.sb(es1, "wg", [128, KC, D], BF16)
            wp = k.sb(es1, "wp", [128, 2, D], BF16)
            wr = k.sb(es1, "wr", [128, KC, NE], BF16)
            rb = k.sb(es1, "rb", [128, NE], F32)
            pt = k.sb(es1, "pt", [128, NT, PLE], F32)
            pT = k.sb(es1, "pT", [128, 2, 2, 128], BF16)
            sg = k.sb(es1, "sg", [128, 2, 512], F32)
            tmp = k.sb(es1, "tmp", [128, 2, 512], F32)
            sc = k.sb(es1, "sc", [128, NT, NE], F32)
            rt = k.sb(es1, "rt", [128, 8, NT * NE], F32)

            ph = Phase(k, "ffn1")
            s_w = ph.sig()
            s_p = ph.sig()
            ph.dma("gpsimd", wg[:], wg_d.rearrange("(c p) d -> p c d", p=128), s_w)
            ph.dma("gpsimd", wp[:], wp_d.rearrange("(c p) d -> p c d", p=128), s_w)
            ph.dma("gpsimd", wr[:], wr_d.rearrange("(c p) d -> p c d", p=128), s_w)
            ph.dma("sync", rb[:], rb_d.partition_broadcast(128), s_w)
            pv = p_d.rearrange("(t p) d -> p t d", p=128)
            p_ready = []
            for q in range(4):
                sq = ph.sig()
                p_ready.append((sq, ph.dma("sync", pt[:, q * 4:(q + 1) * 4, :], pv[:, q * 4:(q + 1) * 4, :], sq)))
            ph.wait("tensor", s_w)

            s_tp = ph.sig()
            s_tc = ph.sig()
            s_mm = ph.sig()
            s_sg = ph.sig()
            s_dv = ph.sig()
            s_r = ph.sig()
            bT = [banks[0], banks[6]]
            bR = banks[1]
            bP = [banks[2], banks[3]]
            bG = [banks[4], banks[5]]
            tc_vals = []
            dv_vals = []
            n_it = 0
            for t in range(NT):
                j = t % 2
                ph.wait("tensor", p_ready[t // 4][0], p_ready[t // 4][1])
                if t >= 2:
                    ph.wait("tensor", s_tc, tc_vals[t - 2])
                for c in range(2):
                    fn = lambda e, o=bT[j][:, c * 128:(c + 1) * 128], i=pt[:, t, c * 128:(c + 1) * 128]: e.transpose(o, i, ident[:])
                    v = ph.do("tensor", fn, post=s_tp) if c == 1 else ph.do("tensor", fn)
                ph.wait("scalar", s_tp, v)
                vtc = ph.do("scalar", lambda e, o=pT[:, j, :, :], i=bT[j][:, 0:256].rearrange("p (c n) -> p c n", c=2): e.activation(out=o, in_=i, func=AF.Copy), post=s_tc)
                tc_vals.append(vtc)
                for c in range(KC):
                    fn = lambda e, o=bR[:, t * NE:(t + 1) * NE], l=XT[:, c, t * 128:(t + 1) * 128], r=wr[:, c, :], st=(c == 0), sp=(c == KC - 1): e.matmul(o, lhsT=l, rhs=r, start=st, stop=sp)
                    if c == KC - 1 and t == NT - 1:
                        ph.do("tensor", fn, post=s_r)
                    else:
                        ph.do("tensor", fn)
                ph.wait("tensor", s_tc, vtc)
                for hb in range(2):
                    jj = n_it % 2
                    if n_it >= 2:
                        ph.wait("tensor", s_dv, dv_vals[n_it - 2])
                    for c in range(2):
                        ph.do("tensor", lambda e, o=bP[jj][:, :], l=pT[:, j, c, :], r=wp[:, c, hb * 512:(hb + 1) * 512], st=(c == 0), sp=(c == 1): e.matmul(o, lhsT=l, rhs=r, start=st, stop=sp))
                    for c in range(KC):
                        fn = lambda e, o=bG[jj][:, :], l=XT[:, c, t * 128:(t + 1) * 128], r=wg[:, c, hb * 512:(hb + 1) * 512], st=(c == 0), sp=(c == KC - 1): e.matmul(o, lhsT=l, rhs=r, start=st, stop=sp)
                        v = ph.do("tensor", fn, post=s_mm) if c == KC - 1 else ph.do("tensor", fn)
                    ph.wait("scalar", s_mm, v)
                    if n_it >= 2:
                        ph.wait("scalar", s_dv, dv_vals[n_it - 2])
                    vs = ph.do("scalar", lambda e, o=sg[:, jj, :], i=bG[jj][:, :]: e.activation(out=o, in_=i, func=AF.Sigmoid), post=s_sg)
                    ph.wait("vector", s_sg, vs)
                    ph.dos("vector", lambda e, o=tmp[:, jj, :], a=bP[jj][:, :], b=sg[:, jj, :]: e.tensor_tensor(out=o, in0=a, in1=b, op=ALU.mult))
                    xs = X[:, t, hb * 512:(hb + 1) * 512]
                    vd = ph.do("vector", lambda e, o=xs, b=tmp[:, jj, :]: e.scalar_tensor_tensor(out=o, in0=o, scalar=ALPHA, in1=b, op0=ALU.mult, op1=ALU.add), post=s_dv)
                    dv_vals.append(vd)
                    n_it += 1
            ph.wait("scalar", s_r)
            s_sc = ph.sig()
            ph.do("scalar", lambda e: e.activation(out=sc[:].rearrange("p t e -> p (t e)"), in_=bR[:, 0:NT * NE], func=AF.Sigmoid), post=s_sc)
            ph.wait("vector", s_sc)
            ph.wait("vector", s_w)
            W = NT * NE
            G4 = NT * 4

            def v4(ap):
                return ap.rearrange("p (g e) -> p g e", e=4)

            sel = rt[:, 0, :]
            m1 = rt[:, 1, 0:G4]
            mk1 = rt[:, 2, :]
            sel2 = rt[:, 3, :]
            m2 = rt[:, 1, G4:2 * G4]
            mk2 = rt[:, 4, :]
            gs = rt[:, 1, 2 * G4:3 * G4]
            gm = rt[:, 1, 3 * G4:3 * G4 + NT]
            gmask = rt[:, 5, 0:G4]
            den = rt[:, 5, G4:G4 + NT]
            rden = rt[:, 5, G4 + NT:G4 + 2 * NT]
            gun = rt[:, 6, :]
            scf = sc[:].rearrange("p t e -> p (t e)")
            BIG = 1.0e4
            dv = lambda fn: ph.dos("vector", fn)
            dv(lambda e: e.tensor_tensor(out=sel.rearrange("p (t e) -> p t e", e=NE), in0=sc[:], in1=rb[:].unsqueeze(1).to_broadcast([128, NT, NE]), op=ALU.add))
            dv(lambda e: e.tensor_reduce(out=m1, in_=v4(sel), axis=AX.X, op=ALU.max))
            dv(lambda e: e.tensor_tensor(out=v4(mk1), in0=v4(sel), in1=m1.unsqueeze(2).to_broadcast([128, G4, 4]), op=ALU.is_ge))
            dv(lambda e: e.scalar_tensor_tensor(out=sel2, in0=mk1, scalar=-BIG, in1=sel, op0=ALU.mult, op1=ALU.add))
            dv(lambda e: e.tensor_reduce(out=m2, in_=v4(sel2), axis=AX.X, op=ALU.max))
            dv(lambda e: e.tensor_tensor(out=v4(mk2), in0=v4(sel2), in1=m2.unsqueeze(2).to_broadcast([128, G4, 4]), op=ALU.is_ge))
            dv(lambda e: e.tensor_tensor(out=gs, in0=m1, in1=m2, op=ALU.add))
            dv(lambda e: e.tensor_reduce(out=gm, in_=gs.rearrange("p (t g) -> p t g", g=4), axis=AX.X, op=ALU.max))
            dv(lambda e: e.tensor_tensor(out=gmask.rearrange("p (t g) -> p t g", g=4), in0=gs.rearrange("p (t g) -> p t g", g=4),
                                         in1=gm.unsqueeze(2).to_broadcast([128, NT, 4]), op=ALU.is_ge))
            dv(lambda e: e.tensor_tensor(out=mk1, in0=mk1, in1=mk2, op=ALU.add))
            dv(lambda e: e.tensor_tensor(out=v4(mk1), in0=v4(mk1), in1=gmask.unsqueeze(2).to_broadcast([128, G4, 4]), op=ALU.mult))
            dv(lambda e: e.tensor_tensor(out=gun, in0=mk1, in1=scf, op=ALU.mult))
            dv(lambda e: e.tensor_reduce(out=den, in_=gun.rearrange("p (t e) -> p t e", e=NE), axis=AX.X, op=ALU.add))
            dv(lambda e: e.reciprocal(out=rden, in_=den))
            dv(lambda e: e.tensor_tensor(out=gates[:], in0=gun.rearrange("p (t e) -> p t e", e=NE), in1=rden.unsqueeze(2).to_broadcast([128, NT, NE]), op=ALU.mult))
            ph.emit()

        with ExitStack() as es3:
          if 'noexp' not in DBG:
              w1 = [k.sb(es3, f"w1_{j}", [128, KC, DE], BF16) for j in range(2)]
              w3 = [k.sb(es3, f"w3_{j}", [128, KC, DE], BF16) for j in range(2)]
              w2 = [k.sb(es3, f"w2_{j}", [128, 4, D], BF16) for j in range(2)]
              hT = [k.sb(es3, f"hT_{j}", [128, 4, 512], BF16) for j in range(2)]
              sl = [k.sb(es3, f"sl_{j}", [128, 512], BF16) for j in range(2)]
              ph = Phase(k, "experts")
              s_wl = [ph.sig(), ph.sig()]
              s_hp = ph.sig()
              s_si = ph.sig()
              s_hd = ph.sig()
              s_yp = ph.sig()
              s_yd = ph.sig()
              s_we = ph.sig()
              bA = [banks[0], banks[1]]
              bB = [banks[2], banks[3]]
              bY = [banks[4], banks[5]]
              wl_vals = {}
              we_vals = {}

              def load_w(e_):
                  j = e_ % 2
                  if e_ >= 2:
                      ph.wait("gpsimd", s_we, we_vals[e_ - 2])
                  ph.dma("gpsimd", w1[j][:], w1_d[e_].rearrange("(c p) f -> p c f", p=128), s_wl[j])
                  ph.dma("gpsimd", w3[j][:], w3_d[e_].rearrange("(c p) f -> p c f", p=128), s_wl[j])
                  wl_vals[e_] = ph.dma("gpsimd", w2[j][:], w2_d[e_].rearrange("(c p) d -> p c d", p=128), s_wl[j])

              NER = int(os.environ.get("NER", NE))
              steps = [(e_, tb) for e_ in range(NER) for tb in range(4)]
              hd_vals = []
              hcount = 0
              ycount = 0
              yd_vals = []
              hstep_last = {}

              def emit_h(si):
                  nonlocal hcount
                  e_, tb = steps[si]
                  j = e_ % 2
                  hb = si % 2
                  if tb == 0:
                      ph.wait("tensor", s_wl[j], wl_vals[e_])
                  for fc in range(4):
                      jj = hcount % 2
                      if hcount >= 2:
                          ph.wait("tensor", s_hd, hd_vals[hcount - 2])
                      for c in range(KC):
                          ph.do("tensor", lambda e, o=bA[jj][:, :], l=w1[j][:, c, fc * 128:(fc + 1) * 128], r=XT[:, c, tb * 512:(tb + 1) * 512], st=(c == 0), sp=(c == KC - 1): e.matmul(o, lhsT=l, rhs=r, start=st, stop=sp))
                      for c in range(KC):
                          fn = lambda e, o=bB[jj][:, :], l=w3[j][:, c, fc * 128:(fc + 1) * 128], r=XT[:, c, tb * 512:(tb + 1) * 512], st=(c == 0), sp=(c == KC - 1): e.matmul(o, lhsT=l, rhs=r, start=st, stop=sp)
                          v = ph.do("tensor", fn, post=s_hp) if c == KC - 1 else ph.do("tensor", fn)
                      ph.wait("scalar", s_hp, v)
                      if hcount >= 2:
                          ph.wait("scalar", s_hd, hd_vals[hcount - 2])
                      vs = ph.do("scalar", lambda e, o=sl[jj][:, :], i=bA[jj][:, :]: e.activation(out=o, in_=i, func=AF.Silu), post=s_si)
                      ph.wait("vector", s_si, vs)
                      if fc == 0 and si >= 2:
                          ph.wait("vector", s_yp, ylast[si - 2])
                      vd = ph.do("vector", lambda e, o=hT[hb][:, fc, :], a=bB[jj][:, :], b=sl[jj][:, :]: e.tensor_tensor(out=o, in0=a, in1=b, op=ALU.mult), post=s_hd)
                      hd_vals.append(vd)
                      hcount += 1
                  hstep_last[si] = vd

              ylast = {}

              def emit_y(si):
                  nonlocal ycount
                  e_, tb = steps[si]
                  j = e_ % 2
                  hb = si % 2
                  ph.wait("tensor", s_hd, hstep_last[si])
                  for tt in range(4):
                      t = tb * 4 + tt
                      for half in range(2):
                          jj = ycount % 2
                          if ycount >= 2:
                              ph.wait("tensor", s_yd, yd_vals[ycount - 2])
                          for fc in range(4):
                              fn = lambda e, o=bY[jj][:, :], l=hT[hb][:, fc, tt * 128:(tt + 1) * 128], r=w2[j][:, fc, half * 512:(half + 1) * 512], st=(fc == 0), sp=(fc == 3): e.matmul(o, lhsT=l, rhs=r, start=st, stop=sp)
                              if fc == 3:
                                  if tt == 3 and half == 1 and tb == 3:
                                      v = ph.do("tensor", fn, post=s_yp)
                                  else:
                                      v = ph.do("tensor", fn, post=s_yp)
                              else:
                                  ph.do("tensor", fn)
                          ph.wait("vector", s_yp, v)
                          xs = X[:, t, half * 512:(half + 1) * 512]
                          vd = ph.do("vector", lambda e, o=xs, a=bY[jj][:, :], g=gates[:, t, e_:e_ + 1]: e.scalar_tensor_tensor(out=o, in0=a, scalar=g, in1=o, op0=ALU.mult, op1=ALU.add), post=s_yd)
                          yd_vals.append(vd)
                          ycount += 1
                  ylast[si] = v
                  if tb == 3:
                      we_vals[e_] = v

              s_we = s_yp
              load_w(0)
              load_w(1)
              nsteps = len(steps)
              emit_h(0)
              for si in range(nsteps):
                  if si + 1 < nsteps:
                      e_n, tb_n = steps[si + 1]
                      emit_h(si + 1)
                  emit_y(si)
                  e_, tb = steps[si]
                  if tb == 3 and e_ + 2 < NER:
                      load_w(e_ + 2)
              ph.emit()

    if 'noln' not in DBG:
        ln_phase(k, X, XT, ident, gb, stats, banks, dt_in["ln_ffn_g"][layer], dt_in["ln_ffn_b"][layer], do_transpose=not last)


def mixer_s5_pool(k, X, XT, ident, gb, stats, banks, dt_in):
    nc = k.nc
    PI = math.pi
    psall = k.psall
    with ExitStack() as es:
        HT = k.sb(es, "HT", [128, KC, S], BF16)
        CAT = XT
        YG = HT
        f32o = lambda name, shape: k.sb(es, name, shape, F32)
        LRE = f32o("LRE", [128, 16]); LIM = f32o("LIM", [128, 16]); LDT = f32o("LDT", [128, 16])
        BRE = f32o("BRE", [128, 16, 16]); BIM = f32o("BIM", [128, 16, 16])
        CRE = f32o("CRE", [128, 16, 16]); NCIM = f32o("NCIM", [128, 16, 16])
        DCOL = f32o("DCOL", [128, 4])
        gsig = Sig(k, NSEM - 1)
        with ExitStack() as es1:
            win = k.sb(es1, "win", [128, KC, D], BF16)
            ph = Phase(k, "inproj")
            lre_d, lim_d, ldt_d = dt_in["s5_lambda_re"], dt_in["s5_lambda_im"], dt_in["s5_log_dt"]
            dm = lambda o, i: ph.do("sync", lambda e, o=o, i=i: e.dma_start(out=o, in_=i, allow_slow_non_contiguous=True), post=gsig, n=16)
            dm(LRE[:], lre_d.rearrange("(gp g2) p -> (g2 p) gp", g2=2))
            dm(LIM[:], lim_d.rearrange("(gp g2) p -> (g2 p) gp", g2=2))
            ldv = ldt_d.rearrange("(gp g2) -> g2 gp", g2=2)
            for g2 in range(2):
                dm(LDT[g2 * 64:(g2 + 1) * 64, :], ldv[g2].partition_broadcast(64))
            dm(BRE[:], dt_in["s5_b_re"].rearrange("(gp g2) p h -> (g2 p) gp h", g2=2))
            dm(BIM[:], dt_in["s5_b_im"].rearrange("(gp g2) p h -> (g2 p) gp h", g2=2))
            crv = dt_in["s5_c_re"].rearrange("(gp g2) h p -> g2 p gp h", g2=2)
            civ = dt_in["s5_c_im"].rearrange("(gp g2) h p -> g2 p gp h", g2=2)
            for g2 in range(2):
                for gp in range(16):
                    dm(CRE[g2 * 64:(g2 + 1) * 64, gp, :], crv[g2, :, gp, :])
                    dm(NCIM[g2 * 64:(g2 + 1) * 64, gp, :], civ[g2, :, gp, :])
            dm(DCOL[:], dt_in["s5_d"].rearrange("(c p) -> p c", p=128))
            s_w = ph.sig()
            s_mm = ph.sig()
            s_ev = {"scalar": ph.sig(), "vector": ph.sig()}
            ph.dma("gpsimd", win[:], dt_in["ab_w_in"].rearrange("(c p) d -> p c d", p=128), s_w)
            ph.wait("tensor", s_w)
            evs = []
            n = 0
            for oc in range(KC):
                for tb in range(4):
                    jj = n % 2
                    if n >= 2:
                        ph.wait("tensor", evs[n - 2][0], evs[n - 2][1])
                    for c in range(KC):
                        fn = mm(banks[jj][:, :], win[:, c, oc * 128:(oc + 1) * 128], XT[:, c, tb * 512:(tb + 1) * 512], c == 0, c == KC - 1)
                        v = ph.do("tensor", fn, post=s_mm) if c == KC - 1 else ph.do("tensor", fn)
                    eng = "scalar" if n % 2 == 0 else "vector"
                    ph.wait(eng, s_mm, v)
                    o = HT[:, oc, tb * 512:(tb + 1) * 512]
                    if eng == "scalar":
                        fn = lambda e, o=o, i=banks[jj][:, :]: e.activation(out=o, in_=i, func=AF.Copy)
                    else:
                        fn = lambda e, o=o, i=banks[jj][:, :]: e.tensor_copy(out=o, in_=i)
                    evs.append((s_ev[eng], ph.do(eng, fn, post=s_ev[eng])))
                    n += 1
            ph.emit()

        with ExitStack() as es2:
            pw = k.sb(es2, "pw", [128, 4, 128], BF16)
            psc = k.sb(es2, "psc", [128, 4], F32)
            ici = k.sb(es2, "ici", [128, 16], I32)
            ic = k.sb(es2, "ic", [128, 16], F32)
            pA = k.sb(es2, "pA", [128, S], F32)
            pB = k.sb(es2, "pB", [128, S], F32)
            pP = k.sb(es2, "pP", [128, S], BF16)
            ph = Phase(k, "pool")
            sr = Serial(ph)
            sr.dma("gpsimd", pw[:], dt_in["pool_w"].rearrange("g c d -> c g d"))
            for gi in range(4):
                sr.dma("sync", psc[:, gi:gi + 1], dt_in["pool_scale"][gi * 128:(gi + 1) * 128].rearrange("(p o) -> p o", o=1))
            sr.op("gpsimd", lambda e: e.iota(ici[:], pattern=[[1, 16]], base=1, channel_multiplier=0))
            sr.op("vector", lambda e: e.tensor_copy(out=ic[:], in_=ici[:]))
            sr.op("vector", lambda e: e.reciprocal(out=ic[:], in_=ic[:]))
            for gi, w in enumerate((2, 4, 8, 16)):
                v = HT[:, 4 + gi, :]
                sr.op("vector", lambda e, v=v: e.tensor_copy(out=pA[:], in_=v))
                a, b = pA, pB
                kk = 1
                while kk < w:
                    sr.op("vector", lambda e, a=a, b=b, kk=kk: e.tensor_tensor(out=b[:, kk:S], in0=a[:, kk:S], in1=a[:, 0:S - kk], op=ALU.add))
                    sr.op("vector", lambda e, a=a, b=b, kk=kk: e.tensor_copy(out=b[:, 0:kk], in_=a[:, 0:kk]))
                    a, b = b, a
                    kk *= 2
                sr.op("vector", lambda e, a=a, v=v, w=w: e.scalar_tensor_tensor(out=pP[:], in0=a[:], scalar=1.0 / w, in1=v, op0=ALU.mult, op1=ALU.subtract))
                sr.op("vector", lambda e, a=a, b=b, w=w: e.tensor_tensor(out=b[:, 0:w - 1], in0=a[:, 0:w - 1], in1=ic[:, 0:w - 1], op=ALU.mult))
                sr.op("vector", lambda e, b=b, v=v, w=w: e.tensor_tensor(out=pP[:, 0:w - 1], in0=b[:, 0:w - 1], in1=v[:, 0:w - 1], op=ALU.subtract))
                for tb in range(4):
                    cs = slice(tb * 512, (tb + 1) * 512)
                    sr.op("tensor", mm(banks[0][:, :], pw[:, gi, :], pP[:, cs], True, True))
                    sr.op("scalar", lambda e, o=CAT[:, 4 + gi, cs], sc=psc[:, gi:gi + 1]: e.activation(out=o, in_=banks[0][:, :], func=AF.Identity, scale=sc))
            ph.emit()

        with ExitStack() as es3:
            f32t = lambda name, shape: k.sb(es3, name, shape, F32)
            BBRE = f32t("BBRE", [128, 16, 16]); BBIM = f32t("BBIM", [128, 16, 16])
            DT = f32t("DT", [128, 16]); MM_ = f32t("MM", [128, 16]); TH = f32t("TH", [128, 16])
            T0 = f32t("T0", [128, 16]); T1s = f32t("T1s", [128, 16]); T2s = f32t("T2s", [128, 16])
            CTH = f32t("CTH", [128, 16]); STH = f32t("STH", [128, 16])
            LBR = f32t("LBR", [128, 16]); LBI = f32t("LBI", [128, 16])
            FRE = f32t("FRE", [128, 16]); FIM = f32t("FIM", [128, 16]); DEN = f32t("DEN", [128, 16])
            OFF = f32t("OFF", [128, 16, 4])
            TVi = k.sb(es3, "TVi", [128, 512], I32)
            TVl = f32t("TVl", [128, 512])
            WG = k.sb(es3, "WG", [128, 4, 512], BF16)
            TMPB = f32t("TMPB", [128, 16, 16])
            E2A = f32t("E2A", [128, 2, 1184])
            BBT = k.sb(es3, "BBT", [128, 4, 3, 128], BF16)
            CPbuf = k.sb(es3, "CPbuf", [128, 1312], BF16)
            CP = CPbuf[:, 0:1024].rearrange("p (a v c) -> p a v c", a=4, v=2)
            TA = f32t("TA", [128, 512]); TC = f32t("TC", [128, 512])
            M1 = f32t("M1", [128, 2, 512]); M2 = f32t("M2", [128, 2, 512])
            WS = [f32t("WS0", [128, 2, 512])]
            XS = k.sb(es3, "XS", [128, 2, 512], BF16)
            MB = f32t("MB", [128, 512])
            PRE = f32t("PRE", [128, 512])
            CAR = f32t("CAR", [128, 2])

            ph = Phase(k, "s5")
            s_ldg = ph.sig()
            ph.dma("gpsimd", WG[:], dt_in["s5_w_glu"].rearrange("(c p) n -> p c n", p=128), s_ldg)

            sr = Serial(ph)
            sr.op("gpsimd", lambda e: e.iota(TVi[:], pattern=[[1, 512]], base=1, channel_multiplier=0))
            ph.wait("vector", gsig)
            ph.wait("vector", s_ldg)
            V_ = lambda fn: sr.op("vector", fn)
            A_ = lambda fn: sr.op("scalar", fn)
            tt = lambda o, a, b, op: (lambda e, o=o, a=a, b=b, op=op: e.tensor_tensor(out=o, in0=a, in1=b, op=op))
            ts = lambda o, a, s1, s2, op0, op1=None: (lambda e, o=o, a=a, s1=s1, s2=s2, op0=op0, op1=op1:
                                                    e.tensor_scalar(out=o, in0=a, scalar1=s1, scalar2=s2, op0=op0, op1=op1) if op1 is not None else
                                                    e.tensor_scalar(out=o, in0=a, scalar1=s1, scalar2=None, op0=op0))
            V_(lambda e: e.tensor_copy(out=TVl[:], in_=TVi[:]))
            V_(ts(NCIM[:], NCIM[:], -1.0, None, ALU.mult))
            A_(lambda e: e.activation(out=DT[:], in_=LDT[:], func=AF.Exp))
            V_(tt(T0[:], LRE[:], DT[:], ALU.mult))
            A_(lambda e: e.activation(out=MM_[:], in_=T0[:], func=AF.Exp))
            V_(tt(TH[:], LIM[:], DT[:], ALU.mult))
            halfpi = f32t("halfpi", [128, 1])
            V_(lambda e: e.memset(halfpi[:], 0.5 * PI))
            KIs = k.sb(es3, "KIs", [128, 64], I32)
            KFs = f32t("KFs", [128, 64])
            PS = 3.1415925

            def sincos(y, U, KI, KF, RC, SN, CS):
                V_(ts(U, y, 1.0 / (2.0 * PI), None, ALU.mult))
                V_(lambda e, KI=KI, U=U: e.tensor_copy(out=KI, in_=U))
                V_(lambda e, KI=KI, KF=KF: e.tensor_copy(out=KF, in_=KI))
                V_(lambda e, KF=KF, y=y: e.scalar_tensor_tensor(out=y, in0=KF, scalar=-2.0 * PI, in1=y, op0=ALU.mult, op1=ALU.add))
                V_(ts(y, y, -PS, PS, ALU.max, ALU.min))
                V_(ts(KF, y, 0.5 * PI, None, ALU.is_gt))
                V_(lambda e, KF=KF, y=y, RC=RC: e.scalar_tensor_tensor(out=RC, in0=KF, scalar=-2.0 * PI, in1=y, op0=ALU.mult, op1=ALU.add))
                A_(lambda e, SN=SN, y=y: e.activation(out=SN, in_=y, func=AF.Sin))
                A_(lambda e, CS=CS, RC=RC: e.activation(out=CS, in_=RC, func=AF.Sin, bias=halfpi[:, 0:1], scale=1.0))

            V_(lambda e: e.tensor_copy(out=T1s[:], in_=TH[:]))
            sincos(T1s[:], T2s[:], KIs[:, 0:16], KFs[:, 0:16], T2s[:], STH[:], CTH[:])
            V_(tt(LBR[:], MM_[:], CTH[:], ALU.mult))
            V_(tt(LBI[:], MM_[:], STH[:], ALU.mult))
            V_(tt(T0[:], LRE[:], LRE[:], ALU.mult))
            V_(tt(T1s[:], LIM[:], LIM[:], ALU.mult))
            V_(tt(DEN[:], T0[:], T1s[:], ALU.add))
            V_(lambda e: e.reciprocal(out=DEN[:], in_=DEN[:]))
            V_(ts(T2s[:], LBR[:], -1.0, None, ALU.add))
            V_(tt(T0[:], T2s[:], LRE[:], ALU.mult))
            V_(tt(T1s[:], LBI[:], LIM[:], ALU.mult))
            V_(tt(T0[:], T0[:], T1s[:], ALU.add))
            V_(tt(FRE[:], T0[:], DEN[:], ALU.mult))
            V_(tt(T0[:], LBI[:], LRE[:], ALU.mult))
            V_(tt(T1s[:], T2s[:], LIM[:], ALU.mult))
            V_(tt(T0[:], T0[:], T1s[:], ALU.subtract))
            V_(tt(FIM[:], T0[:], DEN[:], ALU.mult))
            fb = lambda F: F[:].unsqueeze(2).to_broadcast([128, 16, 16])
            V_(tt(BBRE[:], BRE[:], fb(FRE), ALU.mult))
            V_(tt(TMPB[:], BIM[:], fb(FIM), ALU.mult))
            V_(tt(BBRE[:], BBRE[:], TMPB[:], ALU.subtract))
            V_(tt(BBIM[:], BIM[:], fb(FRE), ALU.mult))
            V_(tt(TMPB[:], BRE[:], fb(FIM), ALU.mult))
            V_(tt(BBIM[:], BBIM[:], TMPB[:], ALU.add))
            OFFv = OFF[:].rearrange("p g t -> p (g t)")
            for tb in range(4):
                V_(ts(OFF[:, :, tb], TH[:], 512.0 * tb, None, ALU.mult))
            V_(ts(KFs[:, 0:64], OFFv, 1.0 / (2.0 * PI), None, ALU.mult))
            V_(lambda e: e.tensor_copy(out=KIs[:, 0:64], in_=KFs[:, 0:64]))
            V_(lambda e: e.tensor_copy(out=KFs[:, 0:64], in_=KIs[:, 0:64]))
            V_(lambda e: e.scalar_tensor_tensor(out=OFFv, in0=KFs[:, 0:64], scalar=-2.0 * PI, in1=OFFv, op0=ALU.mult, op1=ALU.add))
            KI = k.sb(es3, "KI", [128, 512], I32)
            KF = f32t("KF", [128, 512])

            spe = Stream(ph)
            XS2 = [XS, k.sb(es3, "XSb", [128, 2, 512], BF16)]
            S5C = {"n": 0, "bu_next": None, "m2": None, "cg": [None, None], "cgtb": [None] * 4, "ev": [None] * 4}
            bB = [banks[0], banks[1], banks[2]]
            A1 = psall[:, 0:2, :]
            A2 = psall[:, 1:3, :]
            bZ = banks[3]
            bY = [banks[4], banks[5], banks[6], banks[7]]
            for fc in range(4):
                sr.op("gpsimd", lambda e: e.memset(CPbuf[:], 0.0))
                sr.op("gpsimd", lambda e: e.memset(E2A[:], 0.0))
                for g2 in range(2):
                    rs = slice(g2 * 64, (g2 + 1) * 64)
                    for v_, SRC in enumerate((BBRE, BBIM)):
                        dst = E2A[rs, v_, g2 * 16:g2 * 16 + 4 * 288].rearrange("p (a b) -> p a b", b=288)[:, :, 0:16]
                        V_(lambda e, dst=dst, src=SRC[rs, fc * 4:(fc + 1) * 4, :]: e.tensor_copy(out=dst, in_=src))
                    for v_, SRC in enumerate((CRE, NCIM)):
                        st_ = v_ * 128 + g2 * 16
                        dst = CPbuf[rs, st_:st_ + 4 * 288].rearrange("p (a b) -> p a b", b=288)[:, :, 0:16]
                        V_(lambda e, dst=dst, src=SRC[rs, fc * 4:(fc + 1) * 4, :]: e.tensor_copy(out=dst, in_=src))
                for p0 in (0, 2):
                    fns = []
                    for dp in range(2):
                        for v_ in range(2):
                            fns.append(lambda e, o=bZ[:, (dp * 2 + v_) * 128:(dp * 2 + v_ + 1) * 128], i=E2A[:, v_, (p0 + dp) * 256:(p0 + dp) * 256 + 128]: e.transpose(o, i, ident[:]))
                    sr.group("tensor", fns)
                    bzv = bZ[:, :].rearrange("p (a v n) -> p a v n", a=2, v=2)
                    A_(lambda e, p0=p0, bzv=bzv: e.activation(out=BBT[:, p0:p0 + 2, 0:2, :], in_=bzv, func=AF.Copy))
                    V_(lambda e, p0=p0, bzv=bzv: e.tensor_scalar(out=BBT[:, p0:p0 + 2, 2, :], in0=bzv[:, :, 0, :], scalar1=-1.0, scalar2=None, op0=ALU.mult))
                for pl in range(4):
                    gp = fc * 4 + pl
                    V_(lambda e, gp=gp: e.tensor_copy(out=MB[:], in_=MM_[:, gp:gp + 1].to_broadcast([128, 512])))
                    V_(lambda e, gp=gp: e.tensor_scalar(out=TA[:], in0=TVl[:], scalar1=TH[:, gp:gp + 1], scalar2=None, op0=ALU.mult))
                    sincos(TA[:], TC[:], KI[:], KF[:], TC[:], TA[:], TC[:])
                    cb = TC[:].unsqueeze(1).to_broadcast([128, 2, 512])
                    sb_ = TA[:].unsqueeze(1).to_broadcast([128, 2, 512])
                    for tb in range(4):
                        cs = slice(tb * 512, (tb + 1) * 512)
                        stepi = S5C["n"]
                        xs = XS2[stepi % 2]
                        if S5C["bu_next"] is None:
                            S5C["bu_next"] = spe.emit("tensor", [mm(bB[v_][:, :], BBT[:, pl, v_, :], HT[:, fc, cs], True, True) for v_ in range(3)],
                                                      [Node(*sr.last), S5C["m2"]])
                        bu = S5C["bu_next"]
                        S5C["bu_next"] = None
                        ph.wait("vector", bu.sig, bu.val)
                        V_(tt(M1[:], A1, cb, ALU.mult))
                        m2 = V_(tt(M2[:], A2, sb_, ALU.mult))
                        S5C["m2"] = Node(*m2)
                        nxt = (pl, tb + 1) if tb < 3 else ((pl + 1, 0) if pl < 3 else None)
                        if nxt is not None:
                            ncs = slice(nxt[1] * 512, (nxt[1] + 1) * 512)
                            S5C["bu_next"] = spe.emit("tensor", [mm(bB[v_][:, :], BBT[:, nxt[0], v_, :], HT[:, fc, ncs], True, True) for v_ in range(3)], [S5C["m2"]])
                        V_(tt(M1[:], M1[:], M2[:], ALU.add))
                        for ri in range(2):
                            init = 0.0 if tb == 0 else CAR[:, ri:ri + 1]
                            V_(lambda e, ri=ri, init=init: e.tensor_tensor_scan(out=WS[0][:, ri, :], data0=MB[:], data1=M1[:, ri, :], initial=init, op0=ALU.mult, op1=ALU.add))
                        V_(tt(M1[:], WS[0][:], cb, ALU.mult))
                        V_(tt(M2[:], WS[0][:], sb_, ALU.mult))
                        if S5C["cg"][stepi % 2] is not None:
                            cgp = S5C["cg"][stepi % 2]
                            ph.wait("vector", cgp.sig, cgp.val)
                        V_(tt(xs[:, 0, :], M1[:, 0, :], M2[:, 1, :], ALU.subtract))
                        xn = V_(tt(xs[:, 1, :], M1[:, 1, :], M2[:, 0, :], ALU.add))
                        if tb < 3:
                            V_(tt(CAR[:, 0:1], M1[:, 0, 511:512], M2[:, 1, 511:512], ALU.subtract))
                            V_(tt(CAR[:, 1:2], M1[:, 1, 511:512], M2[:, 0, 511:512], ALU.add))
                        cgn = spe.emit("tensor", [mm(bY[tb][:, :], CP[:, pl, 0, :], xs[:, 0, :], pl == 0, False),
                                                  mm(bY[tb][:, :], CP[:, pl, 1, :], xs[:, 1, :], False, pl == 3)], [Node(*xn), S5C["ev"][tb]])
                        S5C["cg"][stepi % 2] = cgn
                        S5C["cgtb"][tb] = cgn
                        S5C["n"] += 1
                for tb in range(4):
                    cs = slice(tb * 512, (tb + 1) * 512)
                    ph.wait("vector", S5C["cgtb"][tb].sig, S5C["cgtb"][tb].val)
                    pe_ = V_(lambda e, cs=cs, tb=tb, fc=fc: e.scalar_tensor_tensor(out=PRE[:], in0=HT[:, fc, cs], scalar=DCOL[:, fc:fc + 1], in1=bY[tb][:, :], op0=ALU.mult, op1=ALU.add))
                    S5C["ev"][tb] = Node(*pe_)
                    A_(lambda e, cs=cs, fc=fc: e.activation(out=YG[:, 4 + fc, cs], in_=PRE[:], func=AF.Gelu_apprx_tanh))
            for oc in range(4):
                for tb in range(4):
                    cs = slice(tb * 512, (tb + 1) * 512)
                    sr.group("tensor", [mm(bZ[:, :], WG[:, c, oc * 128:(oc + 1) * 128], YG[:, 4 + c, cs], c == 0, c == 3) for c in range(4)])
                    A_(lambda e: e.activation(out=PRE[:], in_=bZ[:, :], func=AF.Sigmoid))
                    V_(lambda e, oc=oc, cs=cs: e.tensor_tensor(out=CAT[:, oc, cs], in0=YG[:, 4 + oc, cs], in1=PRE[:], op=ALU.mult))
            ph.emit()

        outproj_residual_ln(k, X, XT, CAT, dt_in["ab_w_out"], dt_in["ln_mix_g"][0], dt_in["ln_mix_b"][0], ident, gb, stats, banks)


class Serial:
    def __init__(self, ph):
        self.ph = ph
        self.sig = ph.sig()
        self.dsig = ph.sig()
        self.last = None

    def _wait(self, eng):
        if self.last is not None:
            self.ph.wait(eng, self.last[0], self.last[1])

    def op(self, eng, fn):
        self._wait(eng)
        self.last = (self.sig, self.ph.do(eng, fn, post=self.sig))
        return self.last

    def group(self, eng, fns):
        self._wait(eng)
        for f in fns[:-1]:
            self.ph.do(eng, f)
        self.last = (self.sig, self.ph.do(eng, fns[-1], post=self.sig))
        return self.last

    def dma(self, eng, out, in_):
        self._wait(eng)
        self.last = (self.dsig, self.ph.dma(eng, out, in_, self.dsig))
        return self.last


def mm(o, l, r, st, sp):
    return lambda e, o=o, l=l, r=r, st=st, sp=sp: e.matmul(o, lhsT=l, rhs=r, start=st, stop=sp)


def outproj_residual_ln(k, X, XT, CT, w_d, g_d, b_d, ident, gb, stats, banks):
    with ExitStack() as es:
        wo = k.sb(es, "wo", [128, KC, D], BF16)
        ph = Phase(k, "outproj")
        s_w = ph.sig()
        s_mm = ph.sig()
        s_dv = ph.sig()
        ph.dma("gpsimd", wo[:], w_d.rearrange("(c p) d -> p c d", p=128), s_w)
        ph.wait("tensor", s_w)
        dvs = []
        n = 0
        for t in range(NT):
            for hb in range(2):
                jj = n % 2
                if n >= 2:
                    ph.wait("tensor", s_dv, dvs[n - 2])
                for c in range(KC):
                    fn = mm(banks[jj][:, :], CT[:, c, t * 128:(t + 1) * 128], wo[:, c, hb * 512:(hb + 1) * 512], c == 0, c == KC - 1)
                    v = ph.do("tensor", fn, post=s_mm) if c == KC - 1 else ph.do("tensor", fn)
                ph.wait("vector", s_mm, v)
                xs = X[:, t, hb * 512:(hb + 1) * 512]
                dvs.append(ph.do("vector", lambda e, o=xs, b=banks[jj][:, :]: e.scalar_tensor_tensor(out=o, in0=o, scalar=ALPHA, in1=b, op0=ALU.mult, op1=ALU.add), post=s_dv))
                n += 1
        ph.emit()
    ln_phase(k, X, XT, ident, gb, stats, banks, g_d, b_d, do_transpose=True)


class Node:
    __slots__ = ("sig", "val")

    def __init__(self, sig, val):
        self.sig = sig
        self.val = val


class Stream:
    def __init__(self, ph):
        self.ph = ph
        self.sigs = {}

    def emit(self, eng, fns, deps=()):
        if not isinstance(fns, (list, tuple)):
            fns = [fns]
        if eng not in self.sigs:
            self.sigs[eng] = self.ph.sig()
        sig = self.sigs[eng]
        for d in deps:
            if d is None:
                continue
            assert d.val <= d.sig.val, "dependency on a not-yet-emitted op"
            self.ph.wait(eng, d.sig, d.val)
        for f in fns[:-1]:
            self.ph.do(eng, f)
        v = self.ph.do(eng, fns[-1], post=sig)
        return Node(sig, v)


def mixer_attention(k, X, XT, ident, gb, stats, banks, dt_in):
    nc = k.nc
    wqkv = dt_in["sb_w_qkv"].rearrange("(c p) n -> p c n", p=128)
    with ExitStack() as eso:
      OT = k.sb(eso, "OT", [128, KC, S], BF16)
      with ExitStack() as es:
        tri = k.sb(es, "tri", [128, 128], BF16)
        ones = k.sb(es, "ones", [128, 128], BF16)
        mlt = k.sb(es, "mlt", [128, 128], BF16)
        QTn = k.sb(es, "QTn", [128, S], BF16)
        KTh = [k.sb(es, f"KTh{h}", [128, S], BF16) for h in range(2)]
        Vh = [k.sb(es, f"Vh{h}", [128, NT, 128], BF16) for h in range(2)]
        wq = k.sb(es, "wq", [128, 3, KC, 128], BF16)
        NCH = 4
        Eb = [k.sb(es, f"Eb{c}", [128, 512], F32) for c in range(NCH)]
        SP = [k.sb(es, f"SP{c}", [128, 512], BF16) for c in range(NCH)]
        SPs = [k.sb(es, f"SPs{c}", [128, 512], BF16) for c in range(NCH)]
        Wt = [k.sb(es, f"Wt{c}", [128, 512], BF16) for c in range(NCH)]
        bz = [banks[0], banks[1], banks[2], banks[3]]
        bo = [banks[4], banks[5], banks[6], banks[7]]

        ph = Phase(k, "attn_consts")
        sc_ = Stream(ph)
        n0 = sc_.emit("gpsimd", lambda e: e.memset(ones[:], 1.0))
        n1 = sc_.emit("gpsimd", lambda e: e.affine_select(out=tri[:], in_=ones[:], pattern=[[-1, 128]], compare_op=ALU.is_ge,
                                                           fill=0.0, base=0, channel_multiplier=1), [n0])
        sc_.emit("gpsimd", lambda e: e.affine_select(out=mlt[:], in_=ones[:], pattern=[[1, 128]], compare_op=ALU.is_gt,
                                                      fill=0.0, base=0, channel_multiplier=-1), [n1])
        sc_.emit("vector", lambda e: e.memset(KTh[0][64:128, :], 0.0))
        sc_.emit("vector", lambda e: e.memset(KTh[1][0:64, :], 0.0))
        sc_.emit("vector", lambda e: e.memset(Vh[0][:, :, 64:128], 0.0))
        sc_.emit("vector", lambda e: e.memset(Vh[1][:, :, 0:64], 0.0))
        ph.emit()

        for hc in range(int(os.environ.get('NHC', KC))):
            ph = Phase(k, f"attn{hc}")
            sp_ = Stream(ph)
            sd = ph.sig()
            for i3 in range(3):
                vd = ph.dma("gpsimd", wq[:, i3, :, :], wqkv[:, :, i3 * D + hc * 128:i3 * D + (hc + 1) * 128], sd)
            wnode = Node(sd, vd)
            ev = [None] * 4
            pj = []
            n = 0
            for tb in range(4):
                cs = slice(tb * 512, (tb + 1) * 512)
                for which in range(2):
                    bk = banks[n % 4]
                    m = sp_.emit("tensor", [mm(bk[:, :], wq[:, which, c, :], XT[:, c, cs], c == 0, c == KC - 1) for c in range(KC)], [wnode, ev[n % 4]])
                    if which == 0:
                        e2 = sp_.emit("vector", lambda e, o=QTn[:, cs], bk=bk: e.tensor_scalar(out=o, in0=bk[:, :], scalar1=-0.125, scalar2=None, op0=ALU.mult), [m])
                    else:
                        e1 = sp_.emit("vector", lambda e, o=KTh[0][0:64, cs], bk=bk: e.tensor_copy(out=o, in_=bk[0:64, :]), [m])
                        e2 = sp_.emit("vector", lambda e, o=KTh[1][64:128, cs], bk=bk: e.tensor_copy(out=o, in_=bk[64:128, :]), [m, e1])
                        pj.append(e1)
                    ev[n % 4] = e2
                    pj.append(e2)
                    n += 1
            for q4 in range(4):
                bk = banks[n % 4]
                fns = []
                for tt in range(4):
                    t = q4 * 4 + tt
                    fns += [mm(bk[:, tt * 128:(tt + 1) * 128], XT[:, c, t * 128:(t + 1) * 128], wq[:, 2, c, :], c == 0, c == KC - 1) for c in range(KC)]
                m = sp_.emit("tensor", fns, [wnode, ev[n % 4]])
                bv = bk[:, :].rearrange("p (t n) -> p t n", t=4)
                e0 = sp_.emit("vector", lambda e, o=Vh[0][:, q4 * 4:(q4 + 1) * 4, 0:64], i=bv[:, :, 0:64]: e.tensor_copy(out=o, in_=i), [m])
                e1 = sp_.emit("vector", lambda e, o=Vh[1][:, q4 * 4:(q4 + 1) * 4, 64:128], i=bv[:, :, 64:128]: e.tensor_copy(out=o, in_=i), [m, e0])
                ev[n % 4] = e1
                pj += [e0, e1]
                n += 1
            projdone = pj

            NQC = int(os.environ.get('NQC', 4))
            batches = [[(0, 3), (1, 3), (0, 2), (1, 2)], [(0, 1), (1, 1), (0, 0), (1, 0)]]
            streams = [Stream(ph) for _ in range(NCH)]
            prev_oev = [None] * NCH
            first_batch = True
            for batch in batches:
                batch = [(h, qc) for (h, qc) in batch if qc < NQC]
                st = [dict() for _ in range(NCH)]
                maxlen = max([4 * qc + 4 for (_, qc) in batch] + [0])
                for s_ in range(maxlen):
                    act = [(c, h, qc) for c, (h, qc) in enumerate(batch) if s_ < 4 * qc + 4]
                    geo = {}
                    for c, h, qc in act:
                        jmax = 4 * qc + 3
                        j = jmax - s_
                        off = max(0, j - 4 * qc) * 128
                        geo[c] = (j, jmax, off, 512 - off, slice(qc * 512 + off, (qc + 1) * 512), slice(j * 128, (j + 1) * 128), j >= 4 * qc)
                    for c, h, qc in act:
                        if s_ == 0:
                            st[c]["rst"] = streams[c].emit("vector", [lambda e, o=SPs[c][:, :]: e.memset(o, 0.0),
                                                                    lambda e, o=Wt[c][:, 0:384]: e.memset(o, 0.0)], [prev_oev[c]] + (projdone if first_batch else []))
                    for c, h, qc in act:
                        j, jmax, off, N, qs, ks, diag = geo[c]
                        st[c]["zm"] = streams[c].emit("tensor", mm(bz[c][:, 0:N], KTh[h][:, ks], QTn[:, qs], True, False),
                                                      [st[c].get("ew"), st[c]["rst"]] + (projdone if first_batch and s_ == 0 else []))
                    for c, h, qc in act:
                        j, jmax, off, N, qs, ks, diag = geo[c]
                        st[c]["ex"] = streams[c].emit("scalar", lambda e, o=Eb[c][:, 0:N], z=bz[c][:, 0:N]: e.activation(out=o, in_=z, func=AF.Exp, scale=-1.0),
                                                      [st[c]["zm"], st[c].get("ln")])
                    for c, h, qc in act:
                        j, jmax, off, N, qs, ks, diag = geo[c]
                        st[c]["ln"] = streams[c].emit("scalar", lambda e, o=SP[c][:, 0:N], z=Eb[c][:, 0:N]: e.activation(out=o, in_=z, func=AF.Ln, bias=1.0),
                                                      [st[c]["ex"], st[c].get("cg"), st[c].get("sum")])
                        st[c]["al"] = st[c]["ln"]
                    for c, h, qc in act:
                        j, jmax, off, N, qs, ks, diag = geo[c]
                        if diag:
                            st[c]["al"] = streams[c].emit("vector", lambda e, o=SP[c][:, 0:128]: e.tensor_tensor(out=o, in0=o, in1=mlt[:, :], op=ALU.mult), [st[c]["ln"]])
                    for c, h, qc in act:
                        j, jmax, off, N, qs, ks, diag = geo[c]
                        fns = [mm(bz[c][:, 0:N], tri[:, :], SP[c][:, 0:N], False, j == jmax)]
                        if j < jmax:
                            fns.append(mm(bz[c][:, 0:N], ones[:, :], SPs[c][:, off:512], False, True))
                        st[c]["cg"] = streams[c].emit("tensor", fns, [st[c]["al"], st[c].get("sum"), st[c]["rst"], st[c]["ex"]])
                    for c, h, qc in act:
                        j, jmax, off, N, qs, ks, diag = geo[c]
                        st[c]["ew"] = streams[c].emit("scalar", lambda e, o=Wt[c][:, off:512], z=bz[c][:, 0:N]: e.activation(out=o, in_=z, func=AF.Exp, scale=-1.0),
                                                      [st[c]["cg"], st[c].get("pv"), st[c]["rst"]])
                        st[c]["wl"] = st[c]["ew"]
                    for c, h, qc in act:
                        j, jmax, off, N, qs, ks, diag = geo[c]
                        if diag:
                            st[c]["wl"] = streams[c].emit("vector", lambda e, o=Wt[c][:, off:off + 128]: e.tensor_tensor(out=o, in0=o, in1=mlt[:, :], op=ALU.mult), [st[c]["ew"]])
                    for c, h, qc in act:
                        j, jmax, off, N, qs, ks, diag = geo[c]
                        if j > 0:
                            st[c]["sum"] = streams[c].emit("vector", lambda e, o=SPs[c][:, off:512], a_=SP[c][:, 0:N]: e.tensor_tensor(out=o, in0=o, in1=a_, op=ALU.add),
                                                           [st[c]["cg"], st[c]["al"], st[c]["rst"]])
                    for c, h, qc in act:
                        j, jmax, off, N, qs, ks, diag = geo[c]
                        st[c]["pv"] = streams[c].emit("tensor", mm(bo[c][:, 0:512], Vh[h][:, j, :], Wt[c][:, 0:512], j == jmax, j == 0),
                                                      [st[c]["wl"], prev_oev[c] if j == jmax else None])
                    for c, h, qc in act:
                        j, jmax, off, N, qs, ks, diag = geo[c]
                        if j == 0:
                            o_ = OT[h * 64:(h + 1) * 64, hc, qc * 512:(qc + 1) * 512]
                            i_ = bo[c][h * 64:(h + 1) * 64, :]
                            prev_oev[c] = streams[c].emit("vector", lambda e, o_=o_, i_=i_: e.tensor_copy(out=o_, in_=i_), [st[c]["pv"]])
                first_batch = False
            ph.emit()
      outproj_residual_ln(k, X, XT, OT, dt_in["sb_w_out"], dt_in["ln_mix_g"][1], dt_in["ln_mix_b"][1], ident, gb, stats, banks)


_NAMES = ["x", "p", "ab_w_in", "s5_lambda_re", "s5_lambda_im", "s5_log_dt", "s5_b_re", "s5_b_im", "s5_c_re", "s5_c_im",
          "s5_d", "s5_w_glu", "pool_w", "pool_scale", "ab_w_out", "sb_w_qkv", "sb_w_out", "ln_mix_g", "ln_mix_b",
          "ln_ffn_g", "ln_ffn_b", "router_w", "router_bias", "moe_w1", "moe_w3", "moe_w2", "ple_w_proj", "ple_w_gate"]


def make_in_maps(inputs, cores):
    f = lambda a: np.ascontiguousarray(np.asarray(a, dtype=np.float32))
    shared = {
        "ab_w_in": f(inputs["ab_w_in"])[0],
        "s5_lambda_re": f(inputs["s5_lambda_re"])[0],
        "s5_lambda_im": f(inputs["s5_lambda_im"])[0],
        "s5_log_dt": f(inputs["s5_log_dt"])[0],
        "s5_b_re": f(inputs["s5_b_re"])[0],
        "s5_b_im": f(inputs["s5_b_im"])[0],
        "s5_c_re": f(inputs["s5_c_re"])[0],
        "s5_c_im": f(inputs["s5_c_im"])[0],
        "s5_d": f(inputs["s5_d"])[0].reshape(512),
        "s5_w_glu": f(inputs["s5_w_glu"])[0],
        "pool_w": f(inputs["pool_w"])[0],
        "pool_scale": f(inputs["pool_scale"])[0],
        "ab_w_out": f(inputs["ab_w_out"])[0],
        "sb_w_qkv": f(inputs["sb_w_qkv"])[0],
        "sb_w_out": f(inputs["sb_w_out"])[0],
        "ln_mix_g": f(inputs["ln_mix_g"]),
        "ln_mix_b": f(inputs["ln_mix_b"]),
        "ln_ffn_g": f(inputs["ln_ffn_g"]),
        "ln_ffn_b": f(inputs["ln_ffn_b"]),
        "router_w": f(inputs["router_w"]),
        "router_bias": f(inputs["router_bias"]),
        "moe_w1": f(inputs["moe_w1"]),
        "moe_w3": f(inputs["moe_w3"]),
        "moe_w2": f(inputs["moe_w2"]),
        "ple_w_proj": f(inputs["ple_w_proj"]),
        "ple_w_gate": f(inputs["ple_w_gate"]),
    }
    x = f(inputs["x"])
    p = f(inputs["p"])
    maps = []
    for c in cores:
        m = dict(shared)
        m["x"] = np.ascontiguousarray(x[c])
        m["p"] = np.ascontiguousarray(p[:, c])
        maps.append(m)
    return maps


def kernel(**inputs):
    nc = bass.Bass("TRN2", target_bir_lowering=False)
    build_program(nc)
    cores = list(range(8))
    in_maps = make_in_maps(inputs, cores)
    res = run_bass_kernel_spmd(nc, in_maps, core_ids=cores)
    out = np.stack([np.asarray(r["y"], dtype=np.float32) for r in res.results], axis=0)
    return out
```

```python
import math
import os
DBG = os.environ.get('KDBG', '')
from contextlib import ExitStack

import numpy as np
import concourse.bass as bass
import concourse.mybir as mybir
from concourse.bass_utils import run_bass_kernel_spmd

F32 = mybir.dt.float32
BF16 = mybir.dt.bfloat16
I32 = mybir.dt.int32
AF = mybir.ActivationFunctionType
ALU = mybir.AluOpType
AX = mybir.AxisListType

S = 2048
D = 1024
NT = 16
KC = 8
NE = 16
DE = 512
PLE = 256
ALPHA = 4.0 ** 0.25
LN_EPS = 1e-5
ENGS = ("tensor", "vector", "scalar", "gpsimd", "sync")
NSEM = 48


class Sig:
    def __init__(self, k, idx):
        self.k = k
        self.idx = idx

    @property
    def sem(self):
        return self.k.sems[self.idx]

    @property
    def val(self):
        return self.k.counts[self.idx]

    def post(self, n=1):
        self.k.counts[self.idx] += n
        return self.k.counts[self.idx]


class Phase:
    def __init__(self, k, name):
        self.k = k
        self.name = name
        self.ops = {e: [] for e in ENGS}
        self.selfsig = {}
        self.waited = {}
        k.next_sem = 0

    def sig(self):
        i = self.k.next_sem
        self.k.next_sem += 1
        assert i < NSEM, "out of semaphores"
        return Sig(self.k, i)

    def do(self, eng, fn, post=None, n=None):
        if post is not None:
            if n is None:
                n = 1
            v = post.post(n)
            sem = post.sem
            self.ops[eng].append(lambda e, fn=fn, sem=sem, n=n: fn(e).then_inc(sem, n))
            return v
        self.ops[eng].append(fn)
        return None

    def dos(self, eng, fn):
        if eng not in self.selfsig:
            self.selfsig[eng] = self.sig()
        sg = self.selfsig[eng]
        v = self.do(eng, fn, post=sg)
        self.wait(eng, sg, v)
        return v

    def dma(self, eng, out, in_, post):
        return self.do(eng, lambda e, out=out, in_=in_: e.dma_start(out=out, in_=in_), post=post, n=16)

    def wait(self, eng, sig, v=None):
        if v is None:
            v = sig.val
        if v <= 0:
            return
        key = (eng, sig.idx)
        if self.waited.get(key, 0) >= v:
            return
        self.waited[key] = v
        sem = sig.sem
        self.ops[eng].append(lambda e, sem=sem, v=v: e.wait_ge(sem, v))

    def emit(self):
        nc = self.k.nc
        with nc.Block() as b:
            for eng in ENGS:
                lst = self.ops[eng]
                if not lst:
                    continue

                def body(e, lst=lst):
                    for f in lst:
                        f(e)

                getattr(b, eng)(body)


class K:
    def __init__(self, nc, es):
        self.nc = nc
        self.es = es
        self.sems = [es.enter_context(nc.semaphore(f"sm{i}")) for i in range(NSEM)]
        self.counts = [0] * NSEM
        self.next_sem = 0

    def sb(self, es, name, shape, dt):
        self.uid = getattr(self, "uid", 0) + 1
        return es.enter_context(self.nc.sbuf_tensor(f"{name}_u{self.uid}", list(shape), dt))


def transpose_tiles(k, ph, X, XT, tiles, ready, banks, ident, done_sig=None):
    psf_e = {"scalar": ph.sig(), "vector": ph.sig()}
    pst = ph.sig()
    evs = []
    for n, t in enumerate(tiles):
        j = n % 2
        if t in ready:
            ph.wait("tensor", ready[t][0], ready[t][1])
        if n >= 2:
            ph.wait("tensor", evs[n - 2][0], evs[n - 2][1])
        for c in range(KC):
            bank = banks[2 * j + c // 4]
            out = bank[:, (c % 4) * 128:(c % 4 + 1) * 128]
            fn = lambda e, out=out, in_=X[:, t, c * 128:(c + 1) * 128]: e.transpose(out, in_, ident[:])
            if c == KC - 1:
                v = ph.do("tensor", fn, post=pst)
            else:
                ph.do("tensor", fn)
        eng = "scalar" if n % 2 == 0 else "vector"
        ph.wait(eng, pst, v)
        for hb in range(2):
            bank = banks[2 * j + hb]
            out = XT[:, hb * 4:(hb + 1) * 4, t * 128:(t + 1) * 128]
            in_ = bank[:, :].rearrange("p (c n) -> p c n", c=4)
            if eng == "scalar":
                fn = lambda e, out=out, in_=in_: e.activation(out=out, in_=in_, func=AF.Copy)
            else:
                fn = lambda e, out=out, in_=in_: e.tensor_copy(out=out, in_=in_)
            if hb == 1:
                evs.append((psf_e[eng], ph.do(eng, fn, post=psf_e[eng])))
            else:
                ph.do(eng, fn)
    return psf_e, evs


def ln_tiles(k, ph, X, tiles, ready, gb, stats, eps_t):
    s_st = ph.sig()
    s_sq = ph.sig()
    s_act = ph.sig()
    s_pool = ph.sig()
    s_dvgb = ph.sig()
    out_ready = {}
    act_vals = {}
    sq_vals = {}
    deferred = []
    nt = len(tiles)

    def stage_a(n):
        t = tiles[n]
        if t in ready:
            ph.wait("vector", ready[t][0], ready[t][1])
        if n >= 4:
            ph.wait("vector", s_act, act_vals[n - 4])
        st = stats[:, n % 4, :]
        for hb in range(2):
            ph.dos("vector", lambda e, o=st[:, hb * 6:(hb + 1) * 6], i=X[:, t, hb * 512:(hb + 1) * 512]: e.bn_stats(out=o, in_=i))
        va = ph.do("vector", lambda e, o=st[:, 12:14], i=st[:, 0:12]: e.bn_aggr(out=o, in_=i), post=s_st)
        ph.wait("scalar", s_st, va)
        sq_vals[n] = ph.do("scalar", lambda e, o=st[:, 14:15], i=st[:, 13:14]: e.activation(out=o, in_=i, func=AF.Sqrt, bias=eps_t[:, 0:1], scale=1.0), post=s_sq)

    def stage_b(n):
        t = tiles[n]
        st = stats[:, n % 4, :]
        rstd = st[:, 14:15]
        nmr = st[:, 15:16]
        ph.wait("vector", s_sq, sq_vals[n])
        ph.dos("vector", lambda e, o=rstd: e.reciprocal(out=o, in_=o))
        v = ph.do("vector", lambda e, o=nmr, i=st[:, 12:13], r=rstd: e.scalar_tensor_tensor(out=o, in0=i, scalar=-1.0, in1=r, op0=ALU.mult, op1=ALU.mult), post=s_st)
        ph.wait("scalar", s_st, v)
        v = ph.do("scalar", lambda e, o=X[:, t, :], r=rstd, b=nmr: e.activation(out=o, in_=o, func=AF.Identity, bias=b, scale=r), post=s_act)
        act_vals[n] = v
        if n % 2 == 0:
            ph.wait("gpsimd", s_act, v)
            ph.dos("gpsimd", lambda e, o=X[:, t, :], g=gb[:, 0, :]: e.tensor_tensor(out=o, in0=o, in1=g, op=ALU.mult))
            v2 = ph.do("gpsimd", lambda e, o=X[:, t, :], g=gb[:, 1, :]: e.tensor_tensor(out=o, in0=o, in1=g, op=ALU.add), post=s_pool)
            out_ready[t] = (s_pool, v2)
        else:
            def gbops(t=t, v=v):
                ph.wait("vector", s_act, v)
                ph.dos("vector", lambda e, o=X[:, t, :], g=gb[:, 0, :]: e.tensor_tensor(out=o, in0=o, in1=g, op=ALU.mult))
                v2 = ph.do("vector", lambda e, o=X[:, t, :], g=gb[:, 1, :]: e.tensor_tensor(out=o, in0=o, in1=g, op=ALU.add), post=s_dvgb)
                out_ready[t] = (s_dvgb, v2)
            deferred.append(gbops)

    stage_a(0)
    for n in range(nt):
        if n + 1 < nt:
            stage_a(n + 1)
        stage_b(n)
        if n % 2 == 0 and deferred:
            deferred.pop(0)()
    while deferred:
        deferred.pop(0)()
    return out_ready, s_act


def bcast_row(dram_vec_ap, n):
    return dram_vec_ap.partition_broadcast(128)


def build_program(nc, plan=("mix0", "ffn0", "mix1", "ffn1")):
    dt_in = {}

    def din(name, shape):
        dt_in[name] = nc.dram_tensor(name, list(shape), F32, kind="ExternalInput").ap()
        return dt_in[name]

    x_d = din("x", [S, D])
    p_d = din("p", [2, S, PLE])
    ab_w_in = din("ab_w_in", [D, D])
    s5_lre = din("s5_lambda_re", [32, 64])
    s5_lim = din("s5_lambda_im", [32, 64])
    s5_ldt = din("s5_log_dt", [32])
    s5_bre = din("s5_b_re", [32, 64, 16])
    s5_bim = din("s5_b_im", [32, 64, 16])
    s5_cre = din("s5_c_re", [32, 16, 64])
    s5_cim = din("s5_c_im", [32, 16, 64])
    s5_d = din("s5_d", [512])
    s5_wglu = din("s5_w_glu", [512, 512])
    pool_w = din("pool_w", [4, 128, 128])
    pool_scale = din("pool_scale", [512])
    ab_w_out = din("ab_w_out", [D, D])
    sb_w_qkv = din("sb_w_qkv", [D, 3 * D])
    sb_w_out = din("sb_w_out", [D, D])
    ln_mix_g = din("ln_mix_g", [2, D])
    ln_mix_b = din("ln_mix_b", [2, D])
    ln_ffn_g = din("ln_ffn_g", [2, D])
    ln_ffn_b = din("ln_ffn_b", [2, D])
    router_w = din("router_w", [D, NE])
    router_bias = din("router_bias", [NE])
    moe_w1 = din("moe_w1", [2, NE, D, DE])
    moe_w3 = din("moe_w3", [2, NE, D, DE])
    moe_w2 = din("moe_w2", [2, NE, DE, D])
    ple_w_proj = din("ple_w_proj", [2, PLE, D])
    ple_w_gate = din("ple_w_gate", [2, D, D])
    y_d = nc.dram_tensor("y", [S, D], F32, kind="ExternalOutput").ap()

    with ExitStack() as es:
        k = K(nc, es)
        X = k.sb(es, "X", [128, NT, D], F32)
        XT = k.sb(es, "XT", [128, KC, S], BF16)
        ident = k.sb(es, "ident", [128, 128], F32)
        gb = k.sb(es, "gb", [128, 2, D], F32)
        stats = k.sb(es, "stats", [128, 4, 16], F32)
        k.eps_t = k.sb(es, "eps_t", [128, 1], F32)
        psall = es.enter_context(nc.psum_tensor("psall", [128, 8, 512], F32))
        k.psall = psall
        banks = [psall[:, i, :] for i in range(8)]

        ph = Phase(k, "load")
        s_ld = ph.sig()
        s_id = ph.sig()
        ph.dos("gpsimd", lambda e: e.memset(ident[:], 0.0))
        ph.dos("gpsimd", lambda e: e.memset(k.eps_t[:], LN_EPS))
        ph.do("gpsimd", lambda e: e.affine_select(out=ident[:], in_=ident[:], pattern=[[-1, 128]], compare_op=ALU.not_equal,
                                                   fill=1.0, base=0, channel_multiplier=1), post=s_id)
        ready = {}
        xv = x_d.rearrange("(t p) d -> p t d", p=128)
        for q in range(4):
            sq = ph.sig()
            for t in range(q * 4, q * 4 + 4):
                v = ph.dma("sync", X[:, t, :], xv[:, t, :], sq)
            for t in range(q * 4, q * 4 + 4):
                ready[t] = (sq, v)
        ph.wait("tensor", s_id)
        transpose_tiles(k, ph, X, XT, list(range(NT)), ready, banks[0:4], ident)
        ph.emit()
        if 'dumpxt' in DBG:
            dbg_d = nc.dram_tensor("dbg", [128, KC * S], F32, kind="ExternalOutput").ap()
            ph = Phase(k, "dump")
            sd = ph.sig()
            for c in range(KC):
                ph.dma("gpsimd", dbg_d[:, c * S:(c + 1) * S], XT[:, c, :], sd)
            ph.wait("gpsimd", sd)
            ph.emit()

        for step in plan:
            layer = int(step[-1])
            if step.startswith("mix"):
                if layer == 0:
                    mixer_s5_pool(k, X, XT, ident, gb, stats, banks, dt_in)
                else:
                    mixer_attention(k, X, XT, ident, gb, stats, banks, dt_in)
            else:
                ffn_layer(k, layer, X, XT, ident, gb, stats, banks, dt_in, last=(step == plan[-1]))

        ph = Phase(k, "store")
        s_st = ph.sig()
        yv = y_d.rearrange("(t p) d -> p t d", p=128)
        for t in range(NT):
            ph.dma("sync", yv[:, t, :], X[:, t, :], s_st)
        ph.wait("sync", s_st)
        ph.emit()
    return nc


def load_gb(ph, gb, g_d, b_d, sig):
    ph.dma("sync", gb[:, 0, :], g_d.partition_broadcast(128), sig)
    return ph.dma("sync", gb[:, 1, :], b_d.partition_broadcast(128), sig)


def ln_phase(k, X, XT, ident, gb, stats, banks, g_d, b_d, do_transpose):
    ph = Phase(k, "ln")
    s_gb = ph.sig()
    load_gb(ph, gb, g_d, b_d, s_gb)
    ph.wait("gpsimd", s_gb)
    ph.wait("vector", s_gb)
    ready, _ = ln_tiles(k, ph, X, list(range(NT)), {}, gb, stats, k.eps_t)
    ph.emit()
    if do_transpose:
        ph = Phase(k, "lnT")
        transpose_tiles(k, ph, X, XT, list(range(NT)), {}, banks[0:4], ident)
        ph.emit()


def ffn_layer(k, layer, X, XT, ident, gb, stats, banks, dt_in, last):
    nc = k.nc
    p_d = dt_in["p"][layer]
    wp_d = dt_in["ple_w_proj"][layer]
    wg_d = dt_in["ple_w_gate"][layer]
    wr_d = dt_in["router_w"]
    rb_d = dt_in["router_bias"]
    w1_d = dt_in["moe_w1"][layer]
    w3_d = dt_in["moe_w3"][layer]
    w2_d = dt_in["moe_w2"][layer]

    with ExitStack() as es:
        gates = k.sb(es, "gates", [128, NT, NE], F32)
        with ExitStack() as es1:
            wg = k.sb(es1, "wg", [128, KC, D], BF16)
            wp = k.sb(es1, "wp", [128, 2, D], BF16)
            wr = k.sb(es1, "wr", [128, KC, NE], BF16)
            rb = k.sb(es1, "rb", [128, NE], F32)
            pt = k.sb(es1, "pt", [128, NT, PLE], F32)
            pT = k.sb(es1, "pT", [128, 2, 2, 128], BF16)
            sg = k.sb(es1, "sg", [128, 2, 512], F32)
            tmp = k.sb(es1, "tmp", [128, 2, 512], F32)
            sc = k.sb(es1, "sc", [128, NT, NE], F32)
            rt = k.sb(es1, "rt", [128, 8, NT * NE], F32)

            ph = Phase(k, "ffn1")
            s_w = ph.sig()
            s_p = ph.sig()
            ph.dma("gpsimd", wg[:], wg_d.rearrange("(c p) d -> p c d", p=128), s_w)
            ph.dma("gpsimd", wp[:], wp_d.rearrange("(c p) d -> p c d", p=128), s_w)
            ph.dma("gpsimd", wr[:], wr_d.rearrange("(c p) d -> p c d", p=128), s_w)
            ph.dma("sync", rb[:], rb_d.partition_broadcast(128), s_w)
            pv = p_d.rearrange("(t p) d -> p t d", p=128)
            p_ready = []
            for q in range(4):
                sq = ph.sig()
                p_ready.append((sq, ph.dma("sync", pt[:, q * 4:(q + 1) * 4, :], pv[:, q * 4:(q + 1) * 4, :], sq)))
            ph.wait("tensor", s_w)

            s_tp = ph.sig()
            s_tc = ph.sig()
            s_mm = ph.sig()
            s_sg = ph.sig()
            s_dv = ph.sig()
            s_r = ph.sig()
            bT = [banks[0], banks[6]]
            bR = banks[1]
            bP = [banks[2], banks[3]]
            bG = [banks[4], banks[5]]
            tc_vals = []
            dv_vals = []
            n_it = 0
            for t in range(NT):
                j = t % 2
                ph.wait("tensor", p_ready[t // 4][0], p_ready[t // 4][1])
                if t >= 2:
                    ph.wait("tensor", s_tc, tc_vals[t - 2])
                for c in range(2):
                    fn = lambda e, o=bT[j][:, c * 128:(c + 1) * 128], i=pt[:, t, c * 128:(c + 1) * 128]: e.transpose(o, i, ident[:])
                    v = ph.do("tensor", fn, post=s_tp) if c == 1 else ph.do("tensor", fn)
                ph.wait("scalar", s_tp, v)
                vtc = ph.do("scalar", lambda e, o=pT[:, j, :, :], i=bT[j][:, 0:256].rearrange("p (c n) -> p c n", c=2): e.activation(out=o, in_=i, func=AF.Copy), post=s_tc)
                tc_vals.append(vtc)
                for c in range(KC):
                    fn = lambda e, o=bR[:, t * NE:(t + 1) * NE], l=XT[:, c, t * 128:(t + 1) * 128], r=wr[:, c, :], st=(c == 0), sp=(c == KC - 1): e.matmul(o, lhsT=l, rhs=r, start=st, stop=sp)
                    if c == KC - 1 and t == NT - 1:
                        ph.do("tensor", fn, post=s_r)
                    else:
                        ph.do("tensor", fn)
                ph.wait("tensor", s_tc, vtc)
                for hb in range(2):
                    jj = n_it % 2
                    if n_it >= 2:
                        ph.wait("tensor", s_dv, dv_vals[n_it - 2])
                    for c in range(2):
                        ph.do("tensor", lambda e, o=bP[jj][:, :], l=pT[:, j, c, :], r=wp[:, c, hb * 512:(hb + 1) * 512], st=(c == 0), sp=(c == 1): e.matmul(o, lhsT=l, rhs=r, start=st, stop=sp))
                    for c in range(KC):
                        fn = lambda e, o=bG[jj][:, :], l=XT[:, c, t * 128:(t + 1) * 128], r=wg[:, c, hb * 512:(hb + 1) * 512], st=(c == 0), sp=(c == KC - 1): e.matmul(o, lhsT=l, rhs=r, start=st, stop=sp)
                        v = ph.do("tensor", fn, post=s_mm) if c == KC - 1 else ph.do("tensor", fn)
                    ph.wait("scalar", s_mm, v)
                    if n_it >= 2:
                        ph.wait("scalar", s_dv, dv_vals[n_it - 2])
                    vs = ph.do("scalar", lambda e, o=sg[:, jj, :], i=bG[jj][:, :]: e.activation(out=o, in_=i, func=AF.Sigmoid), post=s_sg)
                    ph.wait("vector", s_sg, vs)
                    ph.dos("vector", lambda e, o=tmp[:, jj, :], a=bP[jj][:, :], b=sg[:, jj, :]: e.tensor_tensor(out=o, in0=a, in1=b, op=ALU.mult))
                    xs = X[:, t, hb * 512:(hb + 1) * 512]
                    vd = ph.do("vector", lambda e, o=xs, b=tmp[:, jj, :]: e.scalar_tensor_tensor(out=o, in0=o, scalar=ALPHA, in1=b, op0=ALU.mult, op1=ALU.add), post=s_dv)
                    dv_vals.append(vd)
                    n_it += 1
            ph.wait("scalar", s_r)
            s_sc = ph.sig()
            ph.do("scalar", lambda e: e.activation(out=sc[:].rearrange("p t e -> p (t e)"), in_=bR[:, 0:NT * NE], func=AF.Sigmoid), post=s_sc)
            ph.wait("vector", s_sc)
            ph.wait("vector", s_w)
            W = NT * NE
            G4 = NT * 4

            def v4(ap):
                return ap.rearrange("p (g e) -> p g e", e=4)

            sel = rt[:, 0, :]
            m1 = rt[:, 1, 0:G4]
            mk1 = rt[:, 2, :]
            sel2 = rt[:, 3, :]
            m2 = rt[:, 1, G4:2 * G4]
            mk2 = rt[:, 4, :]
            gs = rt[:, 1, 2 * G4:3 * G4]
            gm = rt[:, 1, 3 * G4:3 * G4 + NT]
            gmask = rt[:, 5, 0:G4]
            den = rt[:, 5, G4:G4 + NT]
            rden = rt[:, 5, G4 + NT:G4 + 2 * NT]
            gun = rt[:, 6, :]
            scf = sc[:].rearrange("p t e -> p (t e)")
            BIG = 1.0e4
            dv = lambda fn: ph.dos("vector", fn)
            dv(lambda e: e.tensor_tensor(out=sel.rearrange("p (t e) -> p t e", e=NE), in0=sc[:], in1=rb[:].unsqueeze(1).to_broadcast([128, NT, NE]), op=ALU.add))
            dv(lambda e: e.tensor_reduce(out=m1, in_=v4(sel), axis=AX.X, op=ALU.max))
            dv(lambda e: e.tensor_tensor(out=v4(mk1), in0=v4(sel), in1=m1.unsqueeze(2).to_broadcast([128, G4, 4]), op=ALU.is_ge))
            dv(lambda e: e.scalar_tensor_tensor(out=sel2, in0=mk1, scalar=-BIG, in1=sel, op0=ALU.mult, op1=ALU.add))
            dv(lambda e: e.tensor_reduce(out=m2, in_=v4(sel2), axis=AX.X, op=ALU.max))
            dv(lambda e: e.tensor_tensor(out=v4(mk2), in0=v4(sel2), in1=m2.unsqueeze(2).to_broadcast([128, G4, 4]), op=ALU.is_ge))
            dv(lambda e: e.tensor_tensor(out=gs, in0=m1, in1=m2, op=ALU.add))
            dv(lambda e: e.tensor_reduce(out=gm, in_=gs.rearrange("p (t g) -> p t g", g=4), axis=AX.X, op=ALU.max))
            dv(lambda e: e.tensor_tensor(out=gmask.rearrange("p (t g) -> p t g", g=4), in0=gs.rearrange("p (t g) -> p t g", g=4),
                                         in1=gm.unsqueeze(2).to_broadcast([128, NT, 4]), op=ALU.is_ge))
            dv(lambda e: e.tensor_tensor(out=mk1, in0=mk1, in1=mk2, op=ALU.add))
            dv(lambda e: e.tensor_tensor(out=v4(mk1), in0=v4(mk1), in1=gmask.unsqueeze(2).to_broadcast([128, G4, 4]), op=ALU.mult))
            dv(lambda e: e.tensor_tensor(out=gun, in0=mk1, in1=scf, op=ALU.mult))
            dv(lambda e: e.tensor_reduce(out=den, in_=gun.rearrange("p (t e) -> p t e", e=NE), axis=AX.X, op=ALU.add))
            dv(lambda e: e.reciprocal(out=rden, in_=den))
            dv(lambda e: e.tensor_tensor(out=gates[:], in0=gun.rearrange("p (t e) -> p t e", e=NE), in1=rden.unsqueeze(2).to_broadcast([128, NT, NE]), op=ALU.mult))
            ph.emit()

        with ExitStack() as es3:
          if 'noexp' not in DBG:
              w1 = [k.sb(es3, f"w1_{j}", [128, KC, DE], BF16) for j in range(2)]
              w3 = [k.sb(es3, f"w3_{j}", [128, KC, DE], BF16) for j in range(2)]
              w2 = [k.sb(es3, f"w2_{j}", [128, 4, D], BF16) for j in range(2)]
              hT = [k.sb(es3, f"hT_{j}", [128, 4, 512], BF16) for j in range(2)]
              sl = [k.sb(es3, f"sl_{j}", [128, 512], BF16) for j in range(2)]
              ph = Phase(k, "experts")
              s_wl = [[ph.sig() for _ in range(3)] for _ in range(2)]
              s_hp = ph.sig()
              s_si = ph.sig()
              s_hd = ph.sig()
              s_yp = ph.sig()
              s_yd = ph.sig()
              s_we = ph.sig()
              bA = [banks[0], banks[1]]
              bB = [banks[2], banks[3]]
              bY = [banks[4], banks[5]]
              wl_vals = {}
              we_vals = {}

              def load_w(e_):
                  j = e_ % 2
                  if e_ >= 2:
                      ph.wait("gpsimd", s_we, we_vals[e_ - 2])
                  wl_vals[e_] = [ph.dma("gpsimd", w1[j][:], w1_d[e_].rearrange("(c p) f -> p c f", p=128), s_wl[j][0]),
                                 ph.dma("gpsimd", w3[j][:], w3_d[e_].rearrange("(c p) f -> p c f", p=128), s_wl[j][1]),
                                 ph.dma("gpsimd", w2[j][:], w2_d[e_].rearrange("(c p) d -> p c d", p=128), s_wl[j][2])]

              NER = int(os.environ.get("NER", NE))
              steps = [(e_, tb) for e_ in range(NER) for tb in range(4)]
              hd_vals = []
              hcount = 0
              ycount = 0
              yd_vals = []
              hstep_last = {}

              def emit_h(si):
                  nonlocal hcount
                  e_, tb = steps[si]
                  j = e_ % 2
                  hb = si % 2
                  if tb == 0:
                      ph.wait("tensor", s_wl[j][0], wl_vals[e_][0])
                  for fc in range(4):
                      jj = hcount % 2
                      if hcount >= 2:
                          ph.wait("tensor", s_hd, hd_vals[hcount - 2])
                      for c in range(KC):
                          ph.do("tensor", lambda e, o=bA[jj][:, :], l=w1[j][:, c, fc * 128:(fc + 1) * 128], r=XT[:, c, tb * 512:(tb + 1) * 512], st=(c == 0), sp=(c == KC - 1): e.matmul(o, lhsT=l, rhs=r, start=st, stop=sp))
                      if tb == 0 and fc == 0:
                          ph.wait("tensor", s_wl[j][1], wl_vals[e_][1])
                      for c in range(KC):
                          fn = lambda e, o=bB[jj][:, :], l=w3[j][:, c, fc * 128:(fc + 1) * 128], r=XT[:, c, tb * 512:(tb + 1) * 512], st=(c == 0), sp=(c == KC - 1): e.matmul(o, lhsT=l, rhs=r, start=st, stop=sp)
                          v = ph.do("tensor", fn, post=s_hp) if c == KC - 1 else ph.do("tensor", fn)
                      ph.wait("scalar", s_hp, v)
                      if hcount >= 2:
                          ph.wait("scalar", s_hd, hd_vals[hcount - 2])
                      vs = ph.do("scalar", lambda e, o=sl[jj][:, :], i=bA[jj][:, :]: e.activation(out=o, in_=i, func=AF.Silu), post=s_si)
                      ph.wait("vector", s_si, vs)
                      if fc == 0 and si >= 2:
                          ph.wait("vector", s_yp, ylast[si - 2])
                      vd = ph.do("vector", lambda e, o=hT[hb][:, fc, :], a=bB[jj][:, :], b=sl[jj][:, :]: e.tensor_tensor(out=o, in0=a, in1=b, op=ALU.mult), post=s_hd)
                      hd_vals.append(vd)
                      hcount += 1
                  hstep_last[si] = vd

              ylast = {}

              def emit_y(si):
                  nonlocal ycount
                  e_, tb = steps[si]
                  j = e_ % 2
                  hb = si % 2
                  ph.wait("tensor", s_hd, hstep_last[si])
                  if tb == 0:
                      ph.wait("tensor", s_wl[j][2], wl_vals[e_][2])
                  for tt in range(4):
                      t = tb * 4 + tt
                      for half in range(2):
                          jj = ycount % 2
                          if ycount >= 2:
                              ph.wait("tensor", s_yd, yd_vals[ycount - 2])
                          for fc in range(4):
                              fn = lambda e, o=bY[jj][:, :], l=hT[hb][:, fc, tt * 128:(tt + 1) * 128], r=w2[j][:, fc, half * 512:(half + 1) * 512], st=(fc == 0), sp=(fc == 3): e.matmul(o, lhsT=l, rhs=r, start=st, stop=sp)
                              if fc == 3:
                                  if tt == 3 and half == 1 and tb == 3:
                                      v = ph.do("tensor", fn, post=s_yp)
                                  else:
                                      v = ph.do("tensor", fn, post=s_yp)
                              else:
                                  ph.do("tensor", fn)
                          ph.wait("vector", s_yp, v)
                          xs = X[:, t, half * 512:(half + 1) * 512]
                          vd = ph.do("vector", lambda e, o=xs, a=bY[jj][:, :], g=gates[:, t, e_:e_ + 1]: e.scalar_tensor_tensor(out=o, in0=a, scalar=g, in1=o, op0=ALU.mult, op1=ALU.add), post=s_yd)
                          yd_vals.append(vd)
                          ycount += 1
                  ylast[si] = v
                  if tb == 3:
                      we_vals[e_] = v

              s_we = s_yp
              load_w(0)
              load_w(1)
              nsteps = len(steps)
              emit_h(0)
              for si in range(nsteps):
                  if si + 1 < nsteps:
                      e_n, tb_n = steps[si + 1]
                      emit_h(si + 1)
                  emit_y(si)
                  e_, tb = steps[si]
                  if tb == 3 and e_ + 2 < NER:
                      load_w(e_ + 2)
              ph.emit()

    if 'noln' not in DBG:
        ln_phase(k, X, XT, ident, gb, stats, banks, dt_in["ln_ffn_g"][layer], dt_in["ln_ffn_b"][layer], do_transpose=not last)


def mixer_s5_pool(k, X, XT, ident, gb, stats, banks, dt_in):
    nc = k.nc
    PI = math.pi
    psall = k.psall
    with ExitStack() as es:
        HT = k.sb(es, "HT", [128, KC, S], BF16)
        CAT = XT
        YG = HT
        f32o = lambda name, shape: k.sb(es, name, shape, F32)
        LRE = f32o("LRE", [128, 16]); LIM = f32o("LIM", [128, 16]); LDT = f32o("LDT", [128, 16])
        BRE = f32o("BRE", [128, 16, 16]); BIM = f32o("BIM", [128, 16, 16])
        CRE = f32o("CRE", [128, 16, 16]); NCIM = f32o("NCIM", [128, 16, 16])
        DCOL = f32o("DCOL", [128, 4])
        gsig = Sig(k, NSEM - 1)
        with ExitStack() as es1:
            win = k.sb(es1, "win", [128, KC, D], BF16)
            ph = Phase(k, "inproj")
            lre_d, lim_d, ldt_d = dt_in["s5_lambda_re"], dt_in["s5_lambda_im"], dt_in["s5_log_dt"]
            dm = lambda o, i: ph.do("sync", lambda e, o=o, i=i: e.dma_start(out=o, in_=i, allow_slow_non_contiguous=True), post=gsig, n=16)
            dm(LRE[:], lre_d.rearrange("(gp g2) p -> (g2 p) gp", g2=2))
            dm(LIM[:], lim_d.rearrange("(gp g2) p -> (g2 p) gp", g2=2))
            ldv = ldt_d.rearrange("(gp g2) -> g2 gp", g2=2)
            for g2 in range(2):
                dm(LDT[g2 * 64:(g2 + 1) * 64, :], ldv[g2].partition_broadcast(64))
            dm(BRE[:], dt_in["s5_b_re"].rearrange("(gp g2) p h -> (g2 p) gp h", g2=2))
            dm(BIM[:], dt_in["s5_b_im"].rearrange("(gp g2) p h -> (g2 p) gp h", g2=2))
            crv = dt_in["s5_c_re"].rearrange("(gp g2) h p -> g2 p gp h", g2=2)
            civ = dt_in["s5_c_im"].rearrange("(gp g2) h p -> g2 p gp h", g2=2)
            for g2 in range(2):
                for gp in range(16):
                    dm(CRE[g2 * 64:(g2 + 1) * 64, gp, :], crv[g2, :, gp, :])
                    dm(NCIM[g2 * 64:(g2 + 1) * 64, gp, :], civ[g2, :, gp, :])
            dm(DCOL[:], dt_in["s5_d"].rearrange("(c p) -> p c", p=128))
            s_w = ph.sig()
            s_mm = ph.sig()
            s_ev = {"scalar": ph.sig(), "vector": ph.sig()}
            winv = dt_in["ab_w_in"].rearrange("(c p) d -> p c d", p=128)
            s_wq = [ph.sig() for _ in range(4)]
            wq_vals = []
            for q_ in range(4):
                wq_vals.append(ph.dma("gpsimd", win[:, :, q_ * 256:(q_ + 1) * 256], winv[:, :, q_ * 256:(q_ + 1) * 256], s_wq[q_]))
            evs = []
            n = 0
            for oc in range(KC):
                ph.wait("tensor", s_wq[oc // 2], wq_vals[oc // 2])
                for tb in range(4):
                    jj = n % 2
                    if n >= 2:
                        ph.wait("tensor", evs[n - 2][0], evs[n - 2][1])
                    for c in range(KC):
                        fn = mm(banks[jj][:, :], win[:, c, oc * 128:(oc + 1) * 128], XT[:, c, tb * 512:(tb + 1) * 512], c == 0, c == KC - 1)
                        v = ph.do("tensor", fn, post=s_mm) if c == KC - 1 else ph.do("tensor", fn)
                    eng = "scalar" if n % 2 == 0 else "vector"
                    ph.wait(eng, s_mm, v)
                    o = HT[:, oc, tb * 512:(tb + 1) * 512]
                    if eng == "scalar":
                        fn = lambda e, o=o, i=banks[jj][:, :]: e.activation(out=o, in_=i, func=AF.Copy)
                    else:
                        fn = lambda e, o=o, i=banks[jj][:, :]: e.tensor_copy(out=o, in_=i)
                    evs.append((s_ev[eng], ph.do(eng, fn, post=s_ev[eng])))
                    n += 1
            ph.emit()

        with ExitStack() as es2:
            pw = k.sb(es2, "pw", [128, 4, 128], BF16)
            psc = k.sb(es2, "psc", [128, 4], F32)
            ici = k.sb(es2, "ici", [128, 16], I32)
            ic = k.sb(es2, "ic", [128, 16], F32)
            pA = k.sb(es2, "pA", [128, S], F32)
            pB = k.sb(es2, "pB", [128, S], F32)
            pP = k.sb(es2, "pP", [128, S], BF16)
            ph = Phase(k, "pool")
            sr = Serial(ph)
            sr.dma("gpsimd", pw[:], dt_in["pool_w"].rearrange("g c d -> c g d"))
            for gi in range(4):
                sr.dma("sync", psc[:, gi:gi + 1], dt_in["pool_scale"][gi * 128:(gi + 1) * 128].rearrange("(p o) -> p o", o=1))
            sr.op("gpsimd", lambda e: e.iota(ici[:], pattern=[[1, 16]], base=1, channel_multiplier=0))
            sr.op("vector", lambda e: e.tensor_copy(out=ic[:], in_=ici[:]))
            sr.op("vector", lambda e: e.reciprocal(out=ic[:], in_=ic[:]))
            for gi, w in enumerate((2, 4, 8, 16)):
                v = HT[:, 4 + gi, :]
                sr.op("vector", lambda e, v=v: e.tensor_copy(out=pA[:], in_=v))
                a, b = pA, pB
                kk = 1
                while kk < w:
                    sr.op("vector", lambda e, a=a, b=b, kk=kk: e.tensor_tensor(out=b[:, kk:S], in0=a[:, kk:S], in1=a[:, 0:S - kk], op=ALU.add))
                    sr.op("vector", lambda e, a=a, b=b, kk=kk: e.tensor_copy(out=b[:, 0:kk], in_=a[:, 0:kk]))
                    a, b = b, a
                    kk *= 2
                sr.op("vector", lambda e, a=a, v=v, w=w: e.scalar_tensor_tensor(out=pP[:], in0=a[:], scalar=1.0 / w, in1=v, op0=ALU.mult, op1=ALU.subtract))
                sr.op("vector", lambda e, a=a, b=b, w=w: e.tensor_tensor(out=b[:, 0:w - 1], in0=a[:, 0:w - 1], in1=ic[:, 0:w - 1], op=ALU.mult))
                sr.op("vector", lambda e, b=b, v=v, w=w: e.tensor_tensor(out=pP[:, 0:w - 1], in0=b[:, 0:w - 1], in1=v[:, 0:w - 1], op=ALU.subtract))
                for tb in range(4):
                    cs = slice(tb * 512, (tb + 1) * 512)
                    sr.op("tensor", mm(banks[0][:, :], pw[:, gi, :], pP[:, cs], True, True))
                    sr.op("scalar", lambda e, o=CAT[:, 4 + gi, cs], sc=psc[:, gi:gi + 1]: e.activation(out=o, in_=banks[0][:, :], func=AF.Identity, scale=sc))
            ph.emit()

        with ExitStack() as es3:
            f32t = lambda name, shape: k.sb(es3, name, shape, F32)
            BBRE = f32t("BBRE", [128, 16, 16]); BBIM = f32t("BBIM", [128, 16, 16])
            DT = f32t("DT", [128, 16]); MM_ = f32t("MM", [128, 16]); TH = f32t("TH", [128, 16])
            T0 = f32t("T0", [128, 16]); T1s = f32t("T1s", [128, 16]); T2s = f32t("T2s", [128, 16])
            CTH = f32t("CTH", [128, 16]); STH = f32t("STH", [128, 16])
            LBR = f32t("LBR", [128, 16]); LBI = f32t("LBI", [128, 16])
            FRE = f32t("FRE", [128, 16]); FIM = f32t("FIM", [128, 16]); DEN = f32t("DEN", [128, 16])
            OFF = f32t("OFF", [128, 16, 4])
            TVi = k.sb(es3, "TVi", [128, 512], I32)
            TVl = f32t("TVl", [128, 512])
            WG = k.sb(es3, "WG", [128, 4, 512], BF16)
            TMPB = f32t("TMPB", [128, 16, 16])
            E2A = f32t("E2A", [128, 2, 1184])
            BBT = k.sb(es3, "BBT", [128, 4, 3, 128], BF16)
            CPbuf = k.sb(es3, "CPbuf", [128, 1312], BF16)
            CP = CPbuf[:, 0:1024].rearrange("p (a v c) -> p a v c", a=4, v=2)
            TA = f32t("TA", [128, 512]); TC = f32t("TC", [128, 512])
            M1 = f32t("M1", [128, 2, 512]); M2 = f32t("M2", [128, 2, 512])
            WS = [f32t("WS0", [128, 2, 512])]
            XS = k.sb(es3, "XS", [128, 2, 512], BF16)
            MB = f32t("MB", [128, 512])
            PRE = f32t("PRE", [128, 512])
            CAR = f32t("CAR", [128, 2])

            ph = Phase(k, "s5")
            s_ldg = ph.sig()
            ph.dma("gpsimd", WG[:], dt_in["s5_w_glu"].rearrange("(c p) n -> p c n", p=128), s_ldg)

            sr = Serial(ph)
            sr.op("gpsimd", lambda e: e.iota(TVi[:], pattern=[[1, 512]], base=1, channel_multiplier=0))
            ph.wait("vector", gsig)
            ph.wait("vector", s_ldg)
            V_ = lambda fn: sr.op("vector", fn)
            A_ = lambda fn: sr.op("scalar", fn)
            tt = lambda o, a, b, op: (lambda e, o=o, a=a, b=b, op=op: e.tensor_tensor(out=o, in0=a, in1=b, op=op))
            ts = lambda o, a, s1, s2, op0, op1=None: (lambda e, o=o, a=a, s1=s1, s2=s2, op0=op0, op1=op1:
                                                    e.tensor_scalar(out=o, in0=a, scalar1=s1, scalar2=s2, op0=op0, op1=op1) if op1 is not None else
                                                    e.tensor_scalar(out=o, in0=a, scalar1=s1, scalar2=None, op0=op0))
            V_(lambda e: e.tensor_copy(out=TVl[:], in_=TVi[:]))
            V_(ts(NCIM[:], NCIM[:], -1.0, None, ALU.mult))
            A_(lambda e: e.activation(out=DT[:], in_=LDT[:], func=AF.Exp))
            V_(tt(T0[:], LRE[:], DT[:], ALU.mult))
            A_(lambda e: e.activation(out=MM_[:], in_=T0[:], func=AF.Exp))
            V_(tt(TH[:], LIM[:], DT[:], ALU.mult))
            halfpi = f32t("halfpi", [128, 1])
            V_(lambda e: e.memset(halfpi[:], 0.5 * PI))
            KIs = k.sb(es3, "KIs", [128, 64], I32)
            KFs = f32t("KFs", [128, 64])
            PS = 3.1415925

            def sincos(y, U, KI, KF, RC, SN, CS):
                V_(ts(U, y, 1.0 / (2.0 * PI), None, ALU.mult))
                V_(lambda e, KI=KI, U=U: e.tensor_copy(out=KI, in_=U))
                V_(lambda e, KI=KI, KF=KF: e.tensor_copy(out=KF, in_=KI))
                V_(lambda e, KF=KF, y=y: e.scalar_tensor_tensor(out=y, in0=KF, scalar=-2.0 * PI, in1=y, op0=ALU.mult, op1=ALU.add))
                V_(ts(y, y, -PS, PS, ALU.max, ALU.min))
                V_(ts(KF, y, 0.5 * PI, None, ALU.is_gt))
                V_(lambda e, KF=KF, y=y, RC=RC: e.scalar_tensor_tensor(out=RC, in0=KF, scalar=-2.0 * PI, in1=y, op0=ALU.mult, op1=ALU.add))
                A_(lambda e, SN=SN, y=y: e.activation(out=SN, in_=y, func=AF.Sin))
                A_(lambda e, CS=CS, RC=RC: e.activation(out=CS, in_=RC, func=AF.Sin, bias=halfpi[:, 0:1], scale=1.0))

            V_(lambda e: e.tensor_copy(out=T1s[:], in_=TH[:]))
            sincos(T1s[:], T2s[:], KIs[:, 0:16], KFs[:, 0:16], T2s[:], STH[:], CTH[:])
            V_(tt(LBR[:], MM_[:], CTH[:], ALU.mult))
            V_(tt(LBI[:], MM_[:], STH[:], ALU.mult))
            V_(tt(T0[:], LRE[:], LRE[:], ALU.mult))
            V_(tt(T1s[:], LIM[:], LIM[:], ALU.mult))
            V_(tt(DEN[:], T0[:], T1s[:], ALU.add))
            V_(lambda e: e.reciprocal(out=DEN[:], in_=DEN[:]))
            V_(ts(T2s[:], LBR[:], -1.0, None, ALU.add))
            V_(tt(T0[:], T2s[:], LRE[:], ALU.mult))
            V_(tt(T1s[:], LBI[:], LIM[:], ALU.mult))
            V_(tt(T0[:], T0[:], T1s[:], ALU.add))
            V_(tt(FRE[:], T0[:], DEN[:], ALU.mult))
            V_(tt(T0[:], LBI[:], LRE[:], ALU.mult))
            V_(tt(T1s[:], T2s[:], LIM[:], ALU.mult))
            V_(tt(T0[:], T0[:], T1s[:], ALU.subtract))
            V_(tt(FIM[:], T0[:], DEN[:], ALU.mult))
            fb = lambda F: F[:].unsqueeze(2).to_broadcast([128, 16, 16])
            V_(tt(BBRE[:], BRE[:], fb(FRE), ALU.mult))
            V_(tt(TMPB[:], BIM[:], fb(FIM), ALU.mult))
            V_(tt(BBRE[:], BBRE[:], TMPB[:], ALU.subtract))
            V_(tt(BBIM[:], BIM[:], fb(FRE), ALU.mult))
            V_(tt(TMPB[:], BRE[:], fb(FIM), ALU.mult))
            V_(tt(BBIM[:], BBIM[:], TMPB[:], ALU.add))
            OFFv = OFF[:].rearrange("p g t -> p (g t)")
            for tb in range(4):
                V_(ts(OFF[:, :, tb], TH[:], 512.0 * tb, None, ALU.mult))
            V_(ts(KFs[:, 0:64], OFFv, 1.0 / (2.0 * PI), None, ALU.mult))
            V_(lambda e: e.tensor_copy(out=KIs[:, 0:64], in_=KFs[:, 0:64]))
            V_(lambda e: e.tensor_copy(out=KFs[:, 0:64], in_=KIs[:, 0:64]))
            V_(lambda e: e.scalar_tensor_tensor(out=OFFv, in0=KFs[:, 0:64], scalar=-2.0 * PI, in1=OFFv, op0=ALU.mult, op1=ALU.add))
            KI = k.sb(es3, "KI", [128, 512], I32)
            KF = f32t("KF", [128, 512])

            spe = Stream(ph)
            XS2 = [XS, k.sb(es3, "XSb", [128, 2, 512], BF16)]
            S5C = {"n": 0, "bu_next": None, "m2": None, "cg": [None, None], "cgtb": [None] * 4, "ev": [None] * 4}
            bB = [banks[0], banks[1], banks[2]]
            A1 = psall[:, 0:2, :]
            A2 = psall[:, 1:3, :]
            bZ = banks[3]
            bY = [banks[4], banks[5], banks[6], banks[7]]
            for fc in range(4):
                sr.op("gpsimd", lambda e: e.memset(CPbuf[:], 0.0))
                sr.op("gpsimd", lambda e: e.memset(E2A[:], 0.0))
                for g2 in range(2):
                    rs = slice(g2 * 64, (g2 + 1) * 64)
                    for v_, SRC in enumerate((BBRE, BBIM)):
                        dst = E2A[rs, v_, g2 * 16:g2 * 16 + 4 * 288].rearrange("p (a b) -> p a b", b=288)[:, :, 0:16]
                        V_(lambda e, dst=dst, src=SRC[rs, fc * 4:(fc + 1) * 4, :]: e.tensor_copy(out=dst, in_=src))
                    for v_, SRC in enumerate((CRE, NCIM)):
                        st_ = v_ * 128 + g2 * 16
                        dst = CPbuf[rs, st_:st_ + 4 * 288].rearrange("p (a b) -> p a b", b=288)[:, :, 0:16]
                        V_(lambda e, dst=dst, src=SRC[rs, fc * 4:(fc + 1) * 4, :]: e.tensor_copy(out=dst, in_=src))
                for p0 in (0, 2):
                    fns = []
                    for dp in range(2):
                        for v_ in range(2):
                            fns.append(lambda e, o=bZ[:, (dp * 2 + v_) * 128:(dp * 2 + v_ + 1) * 128], i=E2A[:, v_, (p0 + dp) * 256:(p0 + dp) * 256 + 128]: e.transpose(o, i, ident[:]))
                    sr.group("tensor", fns)
                    bzv = bZ[:, :].rearrange("p (a v n) -> p a v n", a=2, v=2)
                    A_(lambda e, p0=p0, bzv=bzv: e.activation(out=BBT[:, p0:p0 + 2, 0:2, :], in_=bzv, func=AF.Copy))
                    V_(lambda e, p0=p0, bzv=bzv: e.tensor_scalar(out=BBT[:, p0:p0 + 2, 2, :], in0=bzv[:, :, 0, :], scalar1=-1.0, scalar2=None, op0=ALU.mult))
                for pl in range(4):
                    gp = fc * 4 + pl
                    V_(lambda e, gp=gp: e.tensor_copy(out=MB[:], in_=MM_[:, gp:gp + 1].to_broadcast([128, 512])))
                    V_(lambda e, gp=gp: e.tensor_scalar(out=TA[:], in0=TVl[:], scalar1=TH[:, gp:gp + 1], scalar2=None, op0=ALU.mult))
                    sincos(TA[:], TC[:], KI[:], KF[:], TC[:], TA[:], TC[:])
                    cb = TC[:].unsqueeze(1).to_broadcast([128, 2, 512])
                    sb_ = TA[:].unsqueeze(1).to_broadcast([128, 2, 512])
                    for tb in range(4):
                        cs = slice(tb * 512, (tb + 1) * 512)
                        stepi = S5C["n"]
                        xs = XS2[stepi % 2]
                        if S5C["bu_next"] is None:
                            S5C["bu_next"] = spe.emit("tensor", [mm(bB[v_][:, :], BBT[:, pl, v_, :], HT[:, fc, cs], True, True) for v_ in range(3)],
                                                      [Node(*sr.last), S5C["m2"]])
                        bu = S5C["bu_next"]
                        S5C["bu_next"] = None
                        ph.wait("vector", bu.sig, bu.val)
                        V_(tt(M1[:], A1, cb, ALU.mult))
                        m2 = V_(tt(M2[:], A2, sb_, ALU.mult))
                        S5C["m2"] = Node(*m2)
                        nxt = (pl, tb + 1) if tb < 3 else ((pl + 1, 0) if pl < 3 else None)
                        if nxt is not None:
                            ncs = slice(nxt[1] * 512, (nxt[1] + 1) * 512)
                            S5C["bu_next"] = spe.emit("tensor", [mm(bB[v_][:, :], BBT[:, nxt[0], v_, :], HT[:, fc, ncs], True, True) for v_ in range(3)], [S5C["m2"]])
                        V_(tt(M1[:], M1[:], M2[:], ALU.add))
                        for ri in range(2):
                            init = 0.0 if tb == 0 else CAR[:, ri:ri + 1]
                            V_(lambda e, ri=ri, init=init: e.tensor_tensor_scan(out=WS[0][:, ri, :], data0=MB[:], data1=M1[:, ri, :], initial=init, op0=ALU.mult, op1=ALU.add))
                        V_(tt(M1[:], WS[0][:], cb, ALU.mult))
                        V_(tt(M2[:], WS[0][:], sb_, ALU.mult))
                        if S5C["cg"][stepi % 2] is not None:
                            cgp = S5C["cg"][stepi % 2]
                            ph.wait("vector", cgp.sig, cgp.val)
                        V_(tt(xs[:, 0, :], M1[:, 0, :], M2[:, 1, :], ALU.subtract))
                        xn = V_(tt(xs[:, 1, :], M1[:, 1, :], M2[:, 0, :], ALU.add))
                        if tb < 3:
                            V_(tt(CAR[:, 0:1], M1[:, 0, 511:512], M2[:, 1, 511:512], ALU.subtract))
                            V_(tt(CAR[:, 1:2], M1[:, 1, 511:512], M2[:, 0, 511:512], ALU.add))
                        cgn = spe.emit("tensor", [mm(bY[tb][:, :], CP[:, pl, 0, :], xs[:, 0, :], pl == 0, False),
                                                  mm(bY[tb][:, :], CP[:, pl, 1, :], xs[:, 1, :], False, pl == 3)], [Node(*xn), S5C["ev"][tb]])
                        S5C["cg"][stepi % 2] = cgn
                        S5C["cgtb"][tb] = cgn
                        S5C["n"] += 1
                for tb in range(4):
                    cs = slice(tb * 512, (tb + 1) * 512)
                    ph.wait("vector", S5C["cgtb"][tb].sig, S5C["cgtb"][tb].val)
                    pe_ = V_(lambda e, cs=cs, tb=tb, fc=fc: e.scalar_tensor_tensor(out=PRE[:], in0=HT[:, fc, cs], scalar=DCOL[:, fc:fc + 1], in1=bY[tb][:, :], op0=ALU.mult, op1=ALU.add))
                    S5C["ev"][tb] = Node(*pe_)
                    A_(lambda e, cs=cs, fc=fc: e.activation(out=YG[:, 4 + fc, cs], in_=PRE[:], func=AF.Gelu_apprx_tanh))
            for oc in range(4):
                for tb in range(4):
                    cs = slice(tb * 512, (tb + 1) * 512)
                    sr.group("tensor", [mm(bZ[:, :], WG[:, c, oc * 128:(oc + 1) * 128], YG[:, 4 + c, cs], c == 0, c == 3) for c in range(4)])
                    A_(lambda e: e.activation(out=PRE[:], in_=bZ[:, :], func=AF.Sigmoid))
                    V_(lambda e, oc=oc, cs=cs: e.tensor_tensor(out=CAT[:, oc, cs], in0=YG[:, 4 + oc, cs], in1=PRE[:], op=ALU.mult))
            ph.emit()

        outproj_residual_ln(k, X, XT, CAT, dt_in["ab_w_out"], dt_in["ln_mix_g"][0], dt_in["ln_mix_b"][0], ident, gb, stats, banks)


class Serial:
    def __init__(self, ph):
        self.ph = ph
        self.sig = ph.sig()
        self.dsig = ph.sig()
        self.last = None

    def _wait(self, eng):
        if self.last is not None:
            self.ph.wait(eng, self.last[0], self.last[1])

    def op(self, eng, fn):
        self._wait(eng)
        self.last = (self.sig, self.ph.do(eng, fn, post=self.sig))
        return self.last

    def group(self, eng, fns):
        self._wait(eng)
        for f in fns[:-1]:
            self.ph.do(eng, f)
        self.last = (self.sig, self.ph.do(eng, fns[-1], post=self.sig))
        return self.last

    def dma(self, eng, out, in_):
        self._wait(eng)
        self.last = (self.dsig, self.ph.dma(eng, out, in_, self.dsig))
        return self.last


def mm(o, l, r, st, sp):
    return lambda e, o=o, l=l, r=r, st=st, sp=sp: e.matmul(o, lhsT=l, rhs=r, start=st, stop=sp)


def outproj_residual_ln(k, X, XT, CT, w_d, g_d, b_d, ident, gb, stats, banks):
    with ExitStack() as es:
        wo = k.sb(es, "wo", [128, KC, D], BF16)
        ph = Phase(k, "outproj")
        s_w = ph.sig()
        s_mm = ph.sig()
        s_dv = ph.sig()
        wov = w_d.rearrange("(c p) d -> p c d", p=128)
        s_w2 = ph.sig()
        wv = [ph.dma("gpsimd", wo[:, :, 0:512], wov[:, :, 0:512], s_w), ph.dma("gpsimd", wo[:, :, 512:1024], wov[:, :, 512:1024], s_w2)]
        ws_ = [s_w, s_w2]
        dvs = []
        n = 0
        for t in range(NT):
            for hb in range(2):
                ph.wait("tensor", ws_[hb], wv[hb])
                jj = n % 2
                if n >= 2:
                    ph.wait("tensor", s_dv, dvs[n - 2])
                for c in range(KC):
                    fn = mm(banks[jj][:, :], CT[:, c, t * 128:(t + 1) * 128], wo[:, c, hb * 512:(hb + 1) * 512], c == 0, c == KC - 1)
                    v = ph.do("tensor", fn, post=s_mm) if c == KC - 1 else ph.do("tensor", fn)
                ph.wait("vector", s_mm, v)
                xs = X[:, t, hb * 512:(hb + 1) * 512]
                dvs.append(ph.do("vector", lambda e, o=xs, b=banks[jj][:, :]: e.scalar_tensor_tensor(out=o, in0=o, scalar=ALPHA, in1=b, op0=ALU.mult, op1=ALU.add), post=s_dv))
                n += 1
        ph.emit()
    ln_phase(k, X, XT, ident, gb, stats, banks, g_d, b_d, do_transpose=True)


class Node:
    __slots__ = ("sig", "val")

    def __init__(self, sig, val):
        self.sig = sig
        self.val = val


class Stream:
    def __init__(self, ph):
        self.ph = ph
        self.sigs = {}

    def emit(self, eng, fns, deps=()):
        if not isinstance(fns, (list, tuple)):
            fns = [fns]
        if eng not in self.sigs:
            self.sigs[eng] = self.ph.sig()
        sig = self.sigs[eng]
        for d in deps:
            if d is None:
                continue
            assert d.val <= d.sig.val, "dependency on a not-yet-emitted op"
            self.ph.wait(eng, d.sig, d.val)
        for f in fns[:-1]:
            self.ph.do(eng, f)
        v = self.ph.do(eng, fns[-1], post=sig)
        return Node(sig, v)


def mixer_attention(k, X, XT, ident, gb, stats, banks, dt_in):
    nc = k.nc
    wqkv = dt_in["sb_w_qkv"].rearrange("(c p) n -> p c n", p=128)
    with ExitStack() as eso:
      OT = k.sb(eso, "OT", [128, KC, S], BF16)
      with ExitStack() as es:
        tri = k.sb(es, "tri", [128, 128], BF16)
        ones = k.sb(es, "ones", [128, 128], BF16)
        mlt = k.sb(es, "mlt", [128, 128], BF16)
        QTn = k.sb(es, "QTn", [128, S], BF16)
        KTh = [k.sb(es, f"KTh{h}", [128, S], BF16) for h in range(2)]
        Vh = [k.sb(es, f"Vh{h}", [128, NT, 128], BF16) for h in range(2)]
        wq = k.sb(es, "wq", [128, 3, KC, 128], BF16)
        NCH = 4
        Eb = [k.sb(es, f"Eb{c}", [128, 512], F32) for c in range(NCH)]
        SP = [k.sb(es, f"SP{c}", [128, 512], BF16) for c in range(NCH)]
        SPs = [k.sb(es, f"SPs{c}", [128, 512], BF16) for c in range(NCH)]
        Wt = [k.sb(es, f"Wt{c}", [128, 512], BF16) for c in range(NCH)]
        bz = [banks[0], banks[1], banks[2], banks[3]]
        bo = [banks[4], banks[5], banks[6], banks[7]]

        ph = Phase(k, "attn_consts")
        sc_ = Stream(ph)
        n0 = sc_.emit("gpsimd", lambda e: e.memset(ones[:], 1.0))
        n1 = sc_.emit("gpsimd", lambda e: e.affine_select(out=tri[:], in_=ones[:], pattern=[[-1, 128]], compare_op=ALU.is_ge,
                                                           fill=0.0, base=0, channel_multiplier=1), [n0])
        sc_.emit("gpsimd", lambda e: e.affine_select(out=mlt[:], in_=ones[:], pattern=[[1, 128]], compare_op=ALU.is_gt,
                                                      fill=0.0, base=0, channel_multiplier=-1), [n1])
        sc_.emit("vector", lambda e: e.memset(KTh[0][64:128, :], 0.0))
        sc_.emit("vector", lambda e: e.memset(KTh[1][0:64, :], 0.0))
        sc_.emit("vector", lambda e: e.memset(Vh[0][:, :, 64:128], 0.0))
        sc_.emit("vector", lambda e: e.memset(Vh[1][:, :, 0:64], 0.0))
        ph.emit()

        for hc in range(int(os.environ.get('NHC', KC))):
            ph = Phase(k, f"attn{hc}")
            sp_ = Stream(ph)
            sd = ph.sig()
            for i3 in range(3):
                vd = ph.dma("gpsimd", wq[:, i3, :, :], wqkv[:, :, i3 * D + hc * 128:i3 * D + (hc + 1) * 128], sd)
            wnode = Node(sd, vd)
            ev = [None] * 4
            pj = []
            n = 0
            for tb in range(4):
                cs = slice(tb * 512, (tb + 1) * 512)
                for which in range(2):
                    bk = banks[n % 4]
                    m = sp_.emit("tensor", [mm(bk[:, :], wq[:, which, c, :], XT[:, c, cs], c == 0, c == KC - 1) for c in range(KC)], [wnode, ev[n % 4]])
                    if which == 0:
                        e2 = sp_.emit("vector", lambda e, o=QTn[:, cs], bk=bk: e.tensor_scalar(out=o, in0=bk[:, :], scalar1=-0.125, scalar2=None, op0=ALU.mult), [m])
                    else:
                        e1 = sp_.emit("vector", lambda e, o=KTh[0][0:64, cs], bk=bk: e.tensor_copy(out=o, in_=bk[0:64, :]), [m])
                        e2 = sp_.emit("vector", lambda e, o=KTh[1][64:128, cs], bk=bk: e.tensor_copy(out=o, in_=bk[64:128, :]), [m, e1])
                        pj.append(e1)
                    ev[n % 4] = e2
                    pj.append(e2)
                    n += 1
            for q4 in range(4):
                bk = banks[n % 4]
                fns = []
                for tt in range(4):
                    t = q4 * 4 + tt
                    fns += [mm(bk[:, tt * 128:(tt + 1) * 128], XT[:, c, t * 128:(t + 1) * 128], wq[:, 2, c, :], c == 0, c == KC - 1) for c in range(KC)]
                m = sp_.emit("tensor", fns, [wnode, ev[n % 4]])
                bv = bk[:, :].rearrange("p (t n) -> p t n", t=4)
                e0 = sp_.emit("vector", lambda e, o=Vh[0][:, q4 * 4:(q4 + 1) * 4, 0:64], i=bv[:, :, 0:64]: e.tensor_copy(out=o, in_=i), [m])
                e1 = sp_.emit("vector", lambda e, o=Vh[1][:, q4 * 4:(q4 + 1) * 4, 64:128], i=bv[:, :, 64:128]: e.tensor_copy(out=o, in_=i), [m, e0])
                ev[n % 4] = e1
                pj += [e0, e1]
                n += 1
            projdone = pj

            NQC = int(os.environ.get('NQC', 4))
            batches = [[(0, 3), (1, 3), (0, 2), (1, 2)], [(0, 1), (1, 1), (0, 0), (1, 0)]]
            streams = [Stream(ph) for _ in range(NCH)]
            prev_oev = [None] * NCH
            first_batch = True
            for batch in batches:
                batch = [(h, qc) for (h, qc) in batch if qc < NQC]
                st = [dict() for _ in range(NCH)]
                maxlen = max([4 * qc + 4 for (_, qc) in batch] + [0])
                for s_ in range(maxlen):
                    act = [(c, h, qc) for c, (h, qc) in enumerate(batch) if s_ < 4 * qc + 4]
                    geo = {}
                    for c, h, qc in act:
                        jmax = 4 * qc + 3
                        j = jmax - s_
                        off = max(0, j - 4 * qc) * 128
                        geo[c] = (j, jmax, off, 512 - off, slice(qc * 512 + off, (qc + 1) * 512), slice(j * 128, (j + 1) * 128), j >= 4 * qc)
                    for c, h, qc in act:
                        if s_ == 0:
                            st[c]["rst"] = streams[c].emit("vector", [lambda e, o=SPs[c][:, :]: e.memset(o, 0.0),
                                                                    lambda e, o=Wt[c][:, 0:384]: e.memset(o, 0.0)], [prev_oev[c]] + (projdone if first_batch else []))
                    for c, h, qc in act:
                        j, jmax, off, N, qs, ks, diag = geo[c]
                        st[c]["zm"] = streams[c].emit("tensor", mm(bz[c][:, 0:N], KTh[h][:, ks], QTn[:, qs], True, False),
                                                      [st[c].get("ew"), st[c]["rst"]] + (projdone if first_batch and s_ == 0 else []))
                    for c, h, qc in act:
                        j, jmax, off, N, qs, ks, diag = geo[c]
                        st[c]["ex"] = streams[c].emit("scalar", lambda e, o=Eb[c][:, 0:N], z=bz[c][:, 0:N]: e.activation(out=o, in_=z, func=AF.Exp, scale=-1.0),
                                                      [st[c]["zm"], st[c].get("ln")])
                    for c, h, qc in act:
                        j, jmax, off, N, qs, ks, diag = geo[c]
                        st[c]["ln"] = streams[c].emit("scalar", lambda e, o=SP[c][:, 0:N], z=Eb[c][:, 0:N]: e.activation(out=o, in_=z, func=AF.Ln, bias=1.0),
                                                      [st[c]["ex"], st[c].get("cg"), st[c].get("sum")])
                        st[c]["al"] = st[c]["ln"]
                    for c, h, qc in act:
                        j, jmax, off, N, qs, ks, diag = geo[c]
                        if diag:
                            st[c]["al"] = streams[c].emit("vector", lambda e, o=SP[c][:, 0:128]: e.tensor_tensor(out=o, in0=o, in1=mlt[:, :], op=ALU.mult), [st[c]["ln"]])
                    for c, h, qc in act:
                        j, jmax, off, N, qs, ks, diag = geo[c]
                        fns = [mm(bz[c][:, 0:N], tri[:, :], SP[c][:, 0:N], False, j == jmax)]
                        if j < jmax:
                            fns.append(mm(bz[c][:, 0:N], ones[:, :], SPs[c][:, off:512], False, True))
                        st[c]["cg"] = streams[c].emit("tensor", fns, [st[c]["al"], st[c].get("sum"), st[c]["rst"], st[c]["ex"]])
                    for c, h, qc in act:
                        j, jmax, off, N, qs, ks, diag = geo[c]
                        st[c]["ew"] = streams[c].emit("scalar", lambda e, o=Wt[c][:, off:512], z=bz[c][:, 0:N]: e.activation(out=o, in_=z, func=AF.Exp, scale=-1.0),
                                                      [st[c]["cg"], st[c].get("pv"), st[c]["rst"]])
                        st[c]["wl"] = st[c]["ew"]
                    for c, h, qc in act:
                        j, jmax, off, N, qs, ks, diag = geo[c]
                        if diag:
                            st[c]["wl"] = streams[c].emit("vector", lambda e, o=Wt[c][:, off:off + 128]: e.tensor_tensor(out=o, in0=o, in1=mlt[:, :], op=ALU.mult), [st[c]["ew"]])
                    for c, h, qc in act:
                        j, jmax, off, N, qs, ks, diag = geo[c]
                        if j > 0:
                            st[c]["sum"] = streams[c].emit("vector", lambda e, o=SPs[c][:, off:512], a_=SP[c][:, 0:N]: e.tensor_tensor(out=o, in0=o, in1=a_, op=ALU.add),
                                                           [st[c]["cg"], st[c]["al"], st[c]["rst"]])
                    for c, h, qc in act:
                        j, jmax, off, N, qs, ks, diag = geo[c]
                        st[c]["pv"] = streams[c].emit("tensor", mm(bo[c][:, 0:512], Vh[h][:, j, :], Wt[c][:, 0:512], j == jmax, j == 0),
                                                      [st[c]["wl"], prev_oev[c] if j == jmax else None])
                    for c, h, qc in act:
                        j, jmax, off, N, qs, ks, diag = geo[c]
                        if j == 0:
                            o_ = OT[h * 64:(h + 1) * 64, hc, qc * 512:(qc + 1) * 512]
                            i_ = bo[c][h * 64:(h + 1) * 64, :]
                            prev_oev[c] = streams[c].emit("vector", lambda e, o_=o_, i_=i_: e.tensor_copy(out=o_, in_=i_), [st[c]["pv"]])
                first_batch = False
            ph.emit()
      outproj_residual_ln(k, X, XT, OT, dt_in["sb_w_out"], dt_in["ln_mix_g"][1], dt_in["ln_mix_b"][1], ident, gb, stats, banks)


_NAMES = ["x", "p", "ab_w_in", "s5_lambda_re", "s5_lambda_im", "s5_log_dt", "s5_b_re", "s5_b_im", "s5_c_re", "s5_c_im",
          "s5_d", "s5_w_glu", "pool_w", "pool_scale", "ab_w_out", "sb_w_qkv", "sb_w_out", "ln_mix_g", "ln_mix_b",
          "ln_ffn_g", "ln_ffn_b", "router_w", "router_bias", "moe_w1", "moe_w3", "moe_w2", "ple_w_proj", "ple_w_gate"]


def make_in_maps(inputs, cores):
    f = lambda a: np.ascontiguousarray(np.asarray(a, dtype=np.float32))
    shared = {
        "ab_w_in": f(inputs["ab_w_in"])[0],
        "s5_lambda_re": f(inputs["s5_lambda_re"])[0],
        "s5_lambda_im": f(inputs["s5_lambda_im"])[0],
        "s5_log_dt": f(inputs["s5_log_dt"])[0],
        "s5_b_re": f(inputs["s5_b_re"])[0],
        "s5_b_im": f(inputs["s5_b_im"])[0],
        "s5_c_re": f(inputs["s5_c_re"])[0],
        "s5_c_im": f(inputs["s5_c_im"])[0],
        "s5_d": f(inputs["s5_d"])[0].reshape(512),
        "s5_w_glu": f(inputs["s5_w_glu"])[0],
        "pool_w": f(inputs["pool_w"])[0],
        "pool_scale": f(inputs["pool_scale"])[0],
        "ab_w_out": f(inputs["ab_w_out"])[0],
        "sb_w_qkv": f(inputs["sb_w_qkv"])[0],
        "sb_w_out": f(inputs["sb_w_out"])[0],
        "ln_mix_g": f(inputs["ln_mix_g"]),
        "ln_mix_b": f(inputs["ln_mix_b"]),
        "ln_ffn_g": f(inputs["ln_ffn_g"]),
        "ln_ffn_b": f(inputs["ln_ffn_b"]),
        "router_w": f(inputs["router_w"]),
        "router_bias": f(inputs["router_bias"]),
        "moe_w1": f(inputs["moe_w1"]),
        "moe_w3": f(inputs["moe_w3"]),
        "moe_w2": f(inputs["moe_w2"]),
        "ple_w_proj": f(inputs["ple_w_proj"]),
        "ple_w_gate": f(inputs["ple_w_gate"]),
    }
    x = f(inputs["x"])
    p = f(inputs["p"])
    maps = []
    for c in cores:
        m = dict(shared)
        m["x"] = np.ascontiguousarray(x[c])
        m["p"] = np.ascontiguousarray(p[:, c])
        maps.append(m)
    return maps


def kernel(**inputs):
    nc = bass.Bass("TRN2", target_bir_lowering=False)
    build_program(nc)
    cores = list(range(8))
    in_maps = make_in_maps(inputs, cores)
    res = run_bass_kernel_spmd(nc, in_maps, core_ids=cores)
    out = np.stack([np.asarray(r["y"], dtype=np.float32) for r in res.results], axis=0)
    return out
```
